# Optimizing a Trainium2 kernel written in Bass

```python
import math
import jax, jax.numpy as jnp
from jax import lax
import numpy as np

D_MODEL = 1024
BATCH = 8
SEQ = 4096
DEPTH = 4

RW_HEADS = 8
RW_HEAD = 64
RW_DIM = RW_HEADS * RW_HEAD
DECAY_LORA = 64
AAA_LORA = 64
GATE_LORA = 128
GN_EPS = 64e-5
MLA_HEADS = 8
QK_NOPE = 64
QK_ROPE = 32
V_HEAD = 64
Q_LORA = 384
KV_LORA = 256
MLA_DIM = MLA_HEADS * V_HEAD
ROPE_THETA = 10000.0
Q_BLOCK = 128
SHIFT_DIM = 3 * RW_DIM + DECAY_LORA + AAA_LORA + GATE_LORA
REST_DIM = Q_LORA + KV_LORA + QK_ROPE + 2 * D_MODEL
D_IN = SHIFT_DIM + REST_DIM
D_FF = 2816
N_EXPERTS = 8
TOP_K = 2
D_FF_EXPERT = 3584
EXPERT_BLOCK = 256
ALPHA = (2.0 * DEPTH) ** 0.25
BETA = (8.0 * DEPTH) ** -0.25
LN_EPS = 1e-5
RMS_EPS = 1e-6

kernel_name = 'rwkv7_mla_gated_hybrid_deepnorm_moe'


def split_last(z, sizes):
    idx, acc = [], 0
    for s in sizes[:-1]:
        acc += s
        idx.append(acc)
    return jnp.split(z, idx, axis=-1)


def layer_norm(z):
    zf = z.astype(jnp.float32)
    mu = jnp.mean(zf, -1, keepdims=True)
    var = jnp.mean(jnp.square(zf - mu), -1, keepdims=True)
    return ((zf - mu) * lax.rsqrt(var + LN_EPS)).astype(z.dtype)


def rms_norm(z, w):
    zf = z.astype(jnp.float32)
    y = zf * lax.rsqrt(jnp.mean(zf * zf, -1, keepdims=True) + RMS_EPS)
    return (y * w).astype(z.dtype)


def token_shift(z):
    return jnp.pad(z, ((0, 0), (1, 0), (0, 0)))[:, :-1]


def rope_cos_sin(positions, dtype):
    half = QK_ROPE // 2
    inv_freq = jnp.exp(-math.log(ROPE_THETA) * jnp.arange(half, dtype=jnp.float32) / half)
    ang = positions.astype(jnp.float32)[..., None] * inv_freq
    return jnp.cos(ang).astype(dtype), jnp.sin(ang).astype(dtype)


def apply_rope(z, cos, sin):
    z1, z2 = jnp.split(z, 2, axis=-1)
    return jnp.concatenate([z1 * cos - z2 * sin, z2 * cos + z1 * sin], axis=-1)


def rwkv7_branch(r, k, v, dw, da, dg, decay_w0, decay_up, aaa_a0, aaa_up, gate_up,
                 k_k, k_a, r_k, gn_w, gn_b):
    B, S, _ = r.shape
    f32 = jnp.float32
    w_log = -jax.nn.softplus(-(decay_w0 + jnp.tanh(dw) @ decay_up).astype(f32)) - 0.5
    decay = jnp.exp(-jnp.exp(w_log))
    a = jax.nn.sigmoid((aaa_a0 + da @ aaa_up).astype(f32))
    g = jax.nn.sigmoid(dg) @ gate_up
    heads = lambda z: z.astype(f32).reshape(B, S, RW_HEADS, RW_HEAD)
    kk = heads(k * k_k)
    kk = kk * lax.rsqrt(jnp.maximum(jnp.sum(kk * kk, -1, keepdims=True), 1e-24))
    k_mod = k.astype(f32) * (1.0 + (a - 1.0) * k_a)
    r_h, w_h, k_h, v_h, a_h = heads(r), heads(decay), heads(k_mod), heads(v), heads(a)
    tm = lambda z: jnp.swapaxes(z, 0, 1)

    def step(state, inp):
        r_t, w_t, k_t, v_t, kk_t, a_t = inp
        s_kk = jnp.einsum('bhvk,bhk->bhv', state, kk_t)
        state = (state * w_t[:, :, None, :]
                 - s_kk[..., None] * (kk_t * a_t)[:, :, None, :]
                 + v_t[..., None] * k_t[:, :, None, :])
        y_t = jnp.einsum('bhvk,bhk->bhv', state, r_t)
        return state, y_t

    state0 = jnp.zeros((B, RW_HEADS, RW_HEAD, RW_HEAD), f32)
    _, y = lax.scan(step, state0, (tm(r_h), tm(w_h), tm(k_h), tm(v_h), tm(kk), tm(a_h)))
    y = tm(y)
    mu = jnp.mean(y, -1, keepdims=True)
    var = jnp.mean(jnp.square(y - mu), -1, keepdims=True)
    y = ((y - mu) * lax.rsqrt(var + GN_EPS)).reshape(B, S, RW_DIM) * gn_w + gn_b
    bonus = jnp.sum(r_h * k_h * r_k, -1, keepdims=True) * v_h
    y = (y + bonus.reshape(B, S, RW_DIM)) * g.astype(f32)
    return y.astype(r.dtype)


def mla_branch(c_q, c_kv, k_rope, positions, q_norm_w, w_uq, kv_norm_w, w_ukv):
    B, S, _ = c_q.shape
    f32 = jnp.float32
    q = (rms_norm(c_q, q_norm_w) @ w_uq).reshape(B, S, MLA_HEADS, QK_NOPE + QK_ROPE)
    q_nope, q_rope = q[..., :QK_NOPE], q[..., QK_NOPE:]
    kv = (rms_norm(c_kv, kv_norm_w) @ w_ukv).reshape(B, S, MLA_HEADS, QK_NOPE + V_HEAD)
    k_nope, v = kv[..., :QK_NOPE], kv[..., QK_NOPE:]
    cos, sin = rope_cos_sin(positions, q.dtype)
    q_rope = apply_rope(q_rope, cos[:, :, None, :], sin[:, :, None, :])
    k_rope = apply_rope(k_rope, cos, sin)
    scale = (QK_NOPE + QK_ROPE) ** -0.5
    n_blocks = S // Q_BLOCK
    to_blocks = lambda z: jnp.moveaxis(z.reshape(B, n_blocks, Q_BLOCK, *z.shape[2:]), 1, 0)
    key_idx = jnp.arange(S)

    def attend(args):
        qn, qr, blk = args
        s = (jnp.einsum('bqhd,bkhd->bhqk', qn, k_nope)
             + jnp.einsum('bqhd,bkd->bhqk', qr, k_rope)).astype(f32) * scale
        q_idx = blk * Q_BLOCK + jnp.arange(Q_BLOCK)
        s = jnp.where(key_idx[None, :] <= q_idx[:, None], s, -1e30)
        p = jax.nn.softmax(s, axis=-1).astype(v.dtype)
        return jnp.einsum('bhqk,bkhd->bqhd', p, v)

    out = lax.map(attend, (to_blocks(q_nope), to_blocks(q_rope), jnp.arange(n_blocks)))
    return jnp.moveaxis(out, 0, 1).reshape(B, S, MLA_DIM)


def token_mixer(u, positions, w_in, shift_mu, decay_w0, decay_up, aaa_a0, aaa_up, gate_up,
                k_k, k_a, r_k, gn_w, gn_b, q_norm_w, w_uq, kv_norm_w, w_ukv, p_a, p_b, w_o):
    proj = u @ w_in
    p_shift = proj[..., :SHIFT_DIM]
    p_shift = p_shift + (token_shift(p_shift) - p_shift) * shift_mu
    r, k, v, dw, da, dg = split_last(p_shift, [RW_DIM, RW_DIM, RW_DIM, DECAY_LORA, AAA_LORA, GATE_LORA])
    c_q, c_kv, k_rope, gate_a, gate_b = split_last(proj[..., SHIFT_DIM:],
                                                   [Q_LORA, KV_LORA, QK_ROPE, D_MODEL, D_MODEL])
    y_a = rwkv7_branch(r, k, v, dw, da, dg, decay_w0, decay_up, aaa_a0, aaa_up, gate_up,
                       k_k, k_a, r_k, gn_w, gn_b)
    y_b = mla_branch(c_q, c_kv, k_rope, positions, q_norm_w, w_uq, kv_norm_w, w_ukv)
    merged = jax.nn.sigmoid(gate_a) * (y_a @ p_a) + jax.nn.sigmoid(gate_b) * (y_b @ p_b)
    return merged @ w_o


def swiglu(h, w_gate, w_up, w_down):
    return (jax.nn.silu(h @ w_gate) * (h @ w_up)) @ w_down


def moe_swiglu(h, router_w, router_b, w_gate, w_up, w_down):
    B, S, D = h.shape
    N = B * S
    M = N * TOP_K
    ht = h.reshape(N, D)
    logits = (ht @ router_w).astype(jnp.float32) + router_b.astype(jnp.float32)
    top_logit, top_idx = lax.top_k(logits, TOP_K)
    top_gate = jax.nn.softmax(top_logit, axis=-1)
    flat_e = top_idx.reshape(M)
    flat_tok = jnp.arange(M, dtype=jnp.int32) // TOP_K
    flat_gate = top_gate.reshape(M)
    order = jnp.argsort(flat_e)
    e_sorted = flat_e[order]
    counts = jnp.bincount(flat_e, length=N_EXPERTS).astype(jnp.int32)
    padded = (counts + EXPERT_BLOCK - 1) // EXPERT_BLOCK * EXPERT_BLOCK
    start = jnp.cumsum(counts) - counts
    pad_end = jnp.cumsum(padded)
    pad_start = pad_end - padded
    dest = pad_start[e_sorted] + jnp.arange(M, dtype=jnp.int32) - start[e_sorted]
    n_blocks = -(-M // EXPERT_BLOCK) + N_EXPERTS
    m_pad = n_blocks * EXPERT_BLOCK
    slot_tok = jnp.full((m_pad,), N, jnp.int32).at[dest].set(flat_tok[order])
    slot_gate = jnp.zeros((m_pad,), jnp.float32).at[dest].set(flat_gate[order])
    block_e = jnp.minimum(jnp.searchsorted(pad_end, jnp.arange(n_blocks) * EXPERT_BLOCK, side='right'),
                          N_EXPERTS - 1)
    h_ext = jnp.concatenate([ht, jnp.zeros((1, D), ht.dtype)], axis=0)
    xs = h_ext[slot_tok].reshape(n_blocks, EXPERT_BLOCK, D)

    def expert_block(args):
        xb, e = args
        return swiglu(xb, w_gate[e], w_up[e], w_down[e])

    ys = lax.map(expert_block, (xs, block_e)).reshape(m_pad, D)
    ys = ys * slot_gate[:, None].astype(ys.dtype)
    out = jnp.zeros((N + 1, D), ys.dtype).at[slot_tok].add(ys)[:N]
    return out.reshape(B, S, D)


def setup_inputs(seed: int = 0) -> dict:
    key = jax.random.key(seed)
    ks = iter(jax.random.split(key, 48))
    f32 = jnp.float32
    L = DEPTH
    LD = (DEPTH + 1) // 2
    LM = DEPTH // 2
    nrm = lambda shape, s: jax.random.normal(next(ks), shape, f32) * s
    unif = lambda shape, lo, hi: jax.random.uniform(next(ks), shape, f32, lo, hi)
    x = nrm((BATCH, SEQ, D_MODEL), 1.0)
    c = nrm((BATCH, D_MODEL), 1.0)
    positions = (jax.random.randint(next(ks), (BATCH, 1), 0, 1024, jnp.int32)
                 + jnp.arange(SEQ, dtype=jnp.int32)[None, :])
    return {
        'x': x,
        'c': c,
        'positions': positions,
        'w_ada': nrm((L, D_MODEL, 6 * D_MODEL), 0.5 * D_MODEL ** -0.5),
        'b_ada': nrm((L, 6 * D_MODEL), 0.02),
        'w_in': nrm((L, D_MODEL, D_IN), D_MODEL ** -0.5),
        'shift_mu': unif((L, SHIFT_DIM), 0.0, 1.0),
        'decay_w0': unif((L, RW_DIM), -5.0, 0.0),
        'decay_up': nrm((L, DECAY_LORA, RW_DIM), 0.1 * DECAY_LORA ** -0.5),
        'aaa_a0': nrm((L, RW_DIM), 0.1),
        'aaa_up': nrm((L, AAA_LORA, RW_DIM), AAA_LORA ** -0.5),
        'gate_up': nrm((L, GATE_LORA, RW_DIM), GATE_LORA ** -0.5),
        'k_k': 0.85 + nrm((L, RW_DIM), 0.05),
        'k_a': 1.0 + nrm((L, RW_DIM), 0.05),
        'r_k': nrm((L, RW_HEADS, RW_HEAD), 0.1),
        'gn_w': 1.0 + nrm((L, RW_DIM), 0.05),
        'gn_b': nrm((L, RW_DIM), 0.02),
        'q_norm_w': 1.0 + nrm((L, Q_LORA), 0.05),
        'w_uq': nrm((L, Q_LORA, MLA_HEADS * (QK_NOPE + QK_ROPE)), Q_LORA ** -0.5),
        'kv_norm_w': 1.0 + nrm((L, KV_LORA), 0.05),
        'w_ukv': nrm((L, KV_LORA, MLA_HEADS * (QK_NOPE + V_HEAD)), KV_LORA ** -0.5),
        'p_a': nrm((L, RW_DIM, D_MODEL), RW_DIM ** -0.5),
        'p_b': nrm((L, MLA_DIM, D_MODEL), MLA_DIM ** -0.5),
        'w_o': nrm((L, D_MODEL, D_MODEL), BETA * D_MODEL ** -0.5),
        'ln1_w': 1.0 + nrm((L, D_MODEL), 0.05),
        'ln1_b': nrm((L, D_MODEL), 0.02),
        'ln2_w': 1.0 + nrm((L, D_MODEL), 0.05),
        'ln2_b': nrm((L, D_MODEL), 0.02),
        'ffn_w_gate': nrm((LD, D_MODEL, D_FF), D_MODEL ** -0.5),
        'ffn_w_up': nrm((LD, D_MODEL, D_FF), D_MODEL ** -0.5),
        'ffn_w_down': nrm((LD, D_FF, D_MODEL), BETA * D_FF ** -0.5),
        'router_w': nrm((LM, D_MODEL, N_EXPERTS), D_MODEL ** -0.5),
        'router_b': nrm((LM, N_EXPERTS), 0.01),
        'moe_w_gate': nrm((LM, N_EXPERTS, D_MODEL, D_FF_EXPERT), D_MODEL ** -0.5),
        'moe_w_up': nrm((LM, N_EXPERTS, D_MODEL, D_FF_EXPERT), D_MODEL ** -0.5),
        'moe_w_down': nrm((LM, N_EXPERTS, D_FF_EXPERT, D_MODEL), BETA * D_FF_EXPERT ** -0.5),
    }


def reference(x, c, positions, w_ada, b_ada, w_in, shift_mu, decay_w0, decay_up, aaa_a0, aaa_up,
              gate_up, k_k, k_a, r_k, gn_w, gn_b, q_norm_w, w_uq, kv_norm_w, w_ukv, p_a, p_b, w_o,
              ln1_w, ln1_b, ln2_w, ln2_b, ffn_w_gate, ffn_w_up, ffn_w_down, router_w, router_b,
              moe_w_gate, moe_w_up, moe_w_down):
    c_act = jax.nn.silu(c)
    for l in range(DEPTH):
        mod = (c_act @ w_ada[l] + b_ada[l])[:, None, :]
        sh1, sc1, g1, sh2, sc2, g2 = jnp.split(mod, 6, axis=-1)
        u = layer_norm(x) * (1.0 + sc1) + sh1
        mix = token_mixer(u, positions, w_in[l], shift_mu[l], decay_w0[l], decay_up[l], aaa_a0[l],
                          aaa_up[l], gate_up[l], k_k[l], k_a[l], r_k[l], gn_w[l], gn_b[l],
                          q_norm_w[l], w_uq[l], kv_norm_w[l], w_ukv[l], p_a[l], p_b[l], w_o[l])
        x = layer_norm(ALPHA * x + g1 * mix) * ln1_w[l] + ln1_b[l]
        h = layer_norm(x) * (1.0 + sc2) + sh2
        if l % 2 == 0:
            f = swiglu(h, ffn_w_gate[l // 2], ffn_w_up[l // 2], ffn_w_down[l // 2])
        else:
            f = moe_swiglu(h, router_w[l // 2], router_b[l // 2], moe_w_gate[l // 2],
                           moe_w_up[l // 2], moe_w_down[l // 2])
        x = layer_norm(ALPHA * x + g2 * f) * ln2_w[l] + ln2_b[l]
    return x
```

```python
import math
from contextlib import ExitStack
import numpy as np
import concourse.bass as bass
import concourse.mybir as mybir
from concourse.bass_utils import run_bass_kernel_spmd

F32 = mybir.dt.float32
BF16 = mybir.dt.bfloat16
I32 = mybir.dt.int32
AF = mybir.ActivationFunctionType
ALU = mybir.AluOpType
AX = mybir.AxisListType

D = 1024
SH = 1792
DIN = 4512
ALPHA = 8.0 ** 0.25
LN_EPS = 1e-5
RMS_EPS = 1e-6
GN_EPS = 64e-5
DFF = 2816
DFFE = 3584
NE = 8

WSHAPES = [
    ("w_ada", (4, 1024, 6144)), ("b_ada", (4, 6144)), ("w_in", (4, 1024, 4512)), ("shift_mu", (4, 1792)),
    ("decay_w0", (4, 512)), ("decay_up", (4, 64, 512)), ("aaa_a0", (4, 512)), ("aaa_up", (4, 64, 512)),
    ("gate_up", (4, 128, 512)), ("k_k", (4, 512)), ("k_a", (4, 512)), ("r_k", (4, 512)), ("gn_w", (4, 512)),
    ("gn_b", (4, 512)), ("q_norm_w", (4, 384)), ("w_uq", (4, 384, 768)), ("kv_norm_w", (4, 256)),
    ("w_ukv", (4, 256, 1024)), ("p_a", (4, 512, 1024)), ("p_b", (4, 512, 1024)), ("w_o", (4, 1024, 1024)),
    ("ln1_w", (4, 1024)), ("ln1_b", (4, 1024)), ("ln2_w", (4, 1024)), ("ln2_b", (4, 1024)),
    ("ffn_w_gate", (2, 1024, 2816)), ("ffn_w_up", (2, 1024, 2816)), ("ffn_w_down", (2, 2816, 1024)),
    ("router_w", (2, 1024, 8)), ("router_b", (2, 8)),
    ("moe_w_gate", (2, 8, 1024, 3584)), ("moe_w_up", (2, 8, 1024, 3584)), ("moe_w_down", (2, 8, 3584, 1024)),
]


class _StopM2(Exception):
    pass


class TB:
    __slots__ = ("name", "w", "r", "psum")

    def __init__(self, name="", psum=False):
        self.name = name
        self.w = None
        self.r = {}
        self.psum = psum


def PTB(name=""):
    return TB(name, psum=True)


class Sync:
    def __init__(self, nc):
        self.nc = nc
        self.eng = {"pe": nc.tensor, "act": nc.scalar, "dve": nc.vector, "pool": nc.gpsimd, "sp": nc.sync}
        self.sems = {}
        self.count = {}
        self.known = {e: {} for e in self.eng}
        self.ctx = []
        self.nins = 0
        self.rotc = {}

    def sem(self, key):
        if key not in self.sems:
            cm = self.nc.semaphore("s_" + key.replace("#", "_"))
            self.sems[key] = cm.__enter__()
            self.ctx.append(cm)
            self.count[key] = 0
        return self.sems[key]

    def _wait(self, e, need):
        kn = self.known[e]
        for k, v in need.items():
            if kn.get(k, 0) < v:
                self.eng[e].wait_ge(self.sem(k), v)
                kn[k] = v

    def _need(self, e, reads, writes, pe_acc=False):
        need = {}

        def add(ev):
            if ev is None:
                return
            k, v = ev
            if need.get(k, 0) < v:
                need[k] = v
        own = "c_" + e
        for b in reads:
            add(b.w)
            if b.psum:
                for k, v in b.r.items():
                    if k != own:
                        add((k, v))
        for b in writes:
            if not (pe_acc and b.w is not None and b.w[0] == "c_pe"):
                add(b.w)
            for k, v in b.r.items():
                add((k, v))
        self._wait(e, need)

    def _mark(self, ev, reads, writes):
        k, v = ev
        for b in reads:
            if b.r.get(k, 0) < v:
                b.r[k] = v
        for b in writes:
            b.w = ev
            b.r = {}

    def op(self, e, fn, reads=(), writes=(), pe_acc=False):
        self._need(e, reads, writes, pe_acc)
        ins = fn(self.eng[e])
        key = "c_" + e
        s = self.sem(key)
        self.count[key] += 1
        ins.then_inc(s, 1)
        self.nins += 1
        self._mark((key, self.count[key]), reads, writes)

    ROT = {"pre": 3, "const": 3, "ada": 2, "w_m1": 2, "w_m1b": 2, "w_m2": 2, "w_m4": 2, "w_m4b": 2, "w_f": 2,
           "st_psh": 2, "st_gt": 2, "st_qt": 2, "st_kt": 2, "st_va": 2, "st_yat": 2, "st_ybt": 2, "st_x": 2}

    def dma(self, q, out, in_, reads=(), writes=(), key="x", **kw):
        self._need(q, reads, writes)
        rot = self.ROT.get(key, 1)
        key = "d_" + key
        if rot > 1:
            i = self.rotc.get(key, 0)
            self.rotc[key] = i + 1
            key = "%s#%d" % (key, i % rot)
        s = self.sem(key)
        self._wait(q, {key: self.count[key]})
        ins = self.eng[q].dma_start(out=out, in_=in_, **kw)
        self.count[key] += 16
        ins.then_inc(s, 16)
        self.nins += 1
        self._mark((key, self.count[key]), reads, writes)

    def barrier(self, engines=("pe", "act", "dve", "pool", "sp")):
        need = {k: v for k, v in self.count.items() if v > 0}
        for e in engines:
            self._wait(e, need)

    def close(self):
        for cm in reversed(self.ctx):
            cm.__exit__(None, None, None)
        self.ctx = []


class PT:
    def __init__(self, t, p):
        self.t = t
        self.p = p

    def __getitem__(self, key):
        if not isinstance(key, tuple):
            key = (key,)
        k0 = key[0]
        if isinstance(k0, slice) and k0.start is None and k0.stop is None:
            k0 = slice(0, self.p)
        return self.t[(k0,) + tuple(key[1:])]


_UID = [0]


def _uniq(name):
    _UID[0] += 1
    return "%s_u%d" % (name, _UID[0])


def _psum_bank(es, nc, name, shape, dt):
    isz = 4 if dt == F32 else 2
    nfree = 1
    for d in shape[1:]:
        nfree *= d
    assert nfree * isz <= 2048, (name, shape)
    t = es.enter_context(nc.psum_tensor(name, [128, 2048 // isz], dt))
    v = t[:, 0:nfree]
    if len(shape) == 3:
        v = v.rearrange("p (a b) -> p a b", b=shape[2])
    return PT(v, shape[0])


class Ring:
    def __init__(self, es, nc, name, shape, dt, n, psum=False):
        self.t = []
        base = name
        name = _uniq(name)
        for i in range(n):
            if psum:
                t = _psum_bank(es, nc, "%s_%d" % (name, i), shape, dt)
            else:
                t = PT(es.enter_context(nc.sbuf_tensor("%s_%d" % (name, i), [128] + list(shape[1:]), dt)), shape[0])
            self.t.append((t, TB("%s_%d" % (name, i), psum=psum), "%s_%d" % (base, i)))
        self.i = 0

    def next(self):
        r = self.t[self.i % len(self.t)]
        self.i += 1
        return r


def host_consts():
    j = np.arange(128)
    c = {}
    c["identf"] = np.eye(128, dtype=np.float32)
    c["triinc"] = (j[:, None] <= j[None, :]).astype(np.float32)
    c["trisu"] = (j[:, None] > j[None, :]).astype(np.float32)
    strict = (j[:, None] < j[None, :]).astype(np.float32)
    incl = (j[:, None] <= j[None, :]).astype(np.float32)
    c["mAB"] = np.concatenate([-strict, incl], axis=1)
    c["mAK"] = np.concatenate([strict, incl], axis=1)
    c["mN"] = -(j[:, None] > j[None, :]).astype(np.float32)
    q = np.arange(512)
    cm = np.zeros((128, 4, 512), np.float32)
    for jj in range(4):
        cm[:, jj, :] = np.where((jj * 128 + j[:, None]) > q[None, :], -30000.0, 0.0)
    c["cmask"] = cm
    invf = np.exp(-math.log(10000.0) * np.arange(16, dtype=np.float32) / 16).astype(np.float32)
    tab = np.zeros((128, 4), np.float32)
    for p in range(128):
        qq = p % 64
        i = qq % 16
        tab[p, 0] = invf[i] / (2 * math.pi)
        if qq < 32:
            tab[p, 1:] = (0.75, 2 * math.pi, -math.pi)
        elif qq < 48:
            tab[p, 1:] = (0.5, -2 * math.pi, math.pi)
        else:
            tab[p, 1:] = (0.5, 2 * math.pi, -math.pi)
    c["ropetab"] = tab
    return c


CONST_SHAPES = [("identf", (128, 128)), ("triinc", (128, 128)), ("trisu", (128, 128)), ("mAB", (128, 256)),
                ("mAK", (128, 256)), ("mN", (128, 128)), ("cmask", (128, 4, 512)), ("ropetab", (128, 4))]


def build(S_, NL, dbg=()):
    nc = bass.Bass("TRN2", target_bir_lowering=False)
    NT, NG = S_ // 128, S_ // 512
    A = {}

    def din(name, shape, dt=F32):
        A[name] = nc.dram_tensor(name, list(shape), dt, kind="ExternalInput").ap()

    din("x", (S_, D))
    din("c", (8, 128))
    din("pos", (S_,), I32)
    for n, s in WSHAPES:
        din(n, s)
    for n, s in CONST_SHAPES:
        din(n, s)
    Y = nc.dram_tensor("y", [S_, D], F32, kind="ExternalOutput").ap()
    DT = {}

    def scr(name, shape, dt):
        kind = "ExternalOutput" if name in dbg else "Internal"
        A[name] = nc.dram_tensor(name, list(shape), dt, kind=kind).ap()
        DT[name] = TB(name)

    scr("MOD", (NL, 6144), F32)
    scr("XA", (S_, D), F32)
    scr("XB", (S_, D), F32)
    scr("PSH", (S_, SH), F32)
    scr("QT", (8, 96, S_), BF16)
    scr("KT", (8, 96, S_), BF16)
    scr("VA", (8, S_, 128), BF16)
    scr("GT", (2048, S_), F32)
    scr("YAT", (512, S_), BF16)
    scr("YBT", (512, S_), BF16)
    scr("CS", (128, S_), F32)
    scr("WG", (1024, DFF), BF16)
    scr("WU", (1024, DFF), BF16)
    scr("WD", (DFF, 1024), BF16)
    DT["x"] = TB("x")
    DT["y"] = TB("y")

    S = Sync(nc)
    G = ExitStack()

    def sb(es, name, shape, dt=F32):
        return PT(es.enter_context(nc.sbuf_tensor(_uniq(name), [128] + list(shape[1:]), dt)), shape[0])

    def pst(es, name, shape, dt=F32):
        return _psum_bank(es, nc, _uniq(name), shape, dt)

    def mm(out, lhsT, rhs, start, stop, reads, writes):
        S.op("pe", lambda e: e.matmul(out, lhsT, rhs, start=start, stop=stop), reads=reads, writes=writes, pe_acc=True)

    def tr(out, in_, ident, reads, writes):
        S.op("pe", lambda e: e.transpose(out, in_, ident), reads=reads, writes=writes, pe_acc=True)

    def act(out, in_, func, reads, writes, **kw):
        S.op("act", lambda e: e.activation(out, in_, func, **kw), reads=reads, writes=writes)

    def tt(eng, out, in0, in1, op, reads, writes):
        S.op(eng, lambda e: e.tensor_tensor(out, in0, in1, op), reads=reads, writes=writes)

    def ts(eng, out, in0, s1, s2, op0, op1, reads, writes):
        if op1 is None:
            S.op(eng, lambda e: e.tensor_scalar(out, in0, s1, None, op0), reads=reads, writes=writes)
        else:
            S.op(eng, lambda e: e.tensor_scalar(out, in0, s1, s2, op0, op1), reads=reads, writes=writes)

    def stt(out, in0, sc, in1, op0, op1, reads, writes):
        S.op("dve", lambda e: e.scalar_tensor_tensor(out, in0, sc, in1, op0, op1), reads=reads, writes=writes)

    def cp(eng, out, in_, reads, writes):
        if eng == "act":
            act(out, in_, AF.Copy, reads, writes)
        else:
            S.op(eng, lambda e: e.tensor_copy(out, in_), reads=reads, writes=writes)

    CT = {}
    cb = TB("consts")
    for n, s in CONST_SHAPES:
        if n == "cmask":
            CT[n] = sb(G, "c_" + n, s, BF16)
            S.dma("pool", CT[n][:], A[n], writes=[cb], key="constp")
        else:
            CT[n] = sb(G, "c_" + n, s, F32)
            S.dma("sp", CT[n][:], A[n], writes=[cb], key="const")
    identf = CT["identf"]
    identb = sb(G, "identb", (128, 128), BF16)
    onesf = sb(G, "onesf", (128, 128), F32)
    onesb = sb(G, "onesb", (128, 128), BF16)
    modc = sb(G, "modc", (128, 48), F32)
    bmodc = TB("modc")
    cp("dve", identb[:], identf[:], [cb], [cb])
    S.op("dve", lambda e: e.memset(onesf[:], 1.0), writes=[cb])
    S.op("dve", lambda e: e.memset(onesb[:], 1.0), writes=[cb])
    tab = CT["ropetab"]
    def load_cols(es_, vec2d, n, dst, dstb, pT_, pTb, tag):
        tmp = sb(es_, "lc_" + tag, (n, 128), F32)
        tb_ = TB()
        S.dma("sp", tmp[:], vec2d, writes=[tb_], key="lc")
        tr(pT_[:, 0:n], tmp[:, :], identf[0:n, 0:n], [tb_, cb], [pTb])
        cp("dve", dst, pT_[:, 0:n], [pTb], [dstb])

    with ExitStack() as es:
        c8 = sb(es, "c8", (8, 128))
        cT = sb(es, "cT", (128, 8))
        modrow = sb(es, "modrow", (1, 6144))
        brow = sb(es, "brow", (1, 6144))
        wr = Ring(es, nc, "wada", (128, 8, 512), F32, 2)
        pT = pst(es, "ada_pT", (128, 8))
        pr = Ring(es, nc, "ada_ps", (1, 512), F32, 2, psum=True)
        bc8, bcT, bmr, bbr, bpT = TB(), TB(), TB(), TB(), PTB()
        S.dma("sp", c8[:], A["c"], writes=[bc8], key="ada")
        act(c8[:], c8[:], AF.Silu, [bc8], [bc8])
        tr(pT[:, 0:8], c8[:, :], identf[0:8, 0:8], [bc8, cb], [bpT])
        cp("dve", cT[:], pT[:, 0:8], [bpT], [bcT])
        if "DBG" in dbg:
            A["DBG"] = nc.dram_tensor("DBG", [128, 8], F32, kind="ExternalOutput").ap()
            S.dma("sp", A["DBG"], cT[:], reads=[bcT], writes=[TB()], key="dbg")
        for l in range(NL):
            S.dma("sp", brow[:], A["b_ada"][l:l + 1, :], writes=[bbr], key="ada")
            wv = A["w_ada"][l].rearrange("(c p) n -> p c n", p=128)
            for cg in range(12):
                wt, wb, wk = wr.next()
                S.dma("sp", wt[:], wv[:, :, cg * 512:(cg + 1) * 512], writes=[wb], key=wk)
                if "DBG2" in dbg and cg == 0 and l == 0:
                    A["DBG2"] = nc.dram_tensor("DBG2", [128, 8, 512], F32, kind="ExternalOutput").ap()
                    S.dma("sp", A["DBG2"], wt[:], reads=[wb], writes=[TB()], key="dbg")
                ps, pb, _ = pr.next()
                for c in range(8):
                    mm(ps[0:1, :], cT[:, c:c + 1], wt[:, c, :], c == 0, c == 7, [bcT, wb], [pb])
                tt("dve", modrow[0:1, cg * 512:(cg + 1) * 512], ps[0:1, :], brow[0:1, cg * 512:(cg + 1) * 512],
                   ALU.add, [pb, bbr], [bmr])
            S.dma("sp", A["MOD"][l:l + 1, :], modrow[:], reads=[bmr], writes=[DT["MOD"]], key="st_mod")
        S.barrier()

    import os as _os
    for es in ([] if _os.environ.get("MK_NOROPE") else [ExitStack()]):
        pi_ = sb(es, "pos_i", (128, S_), I32)
        pf = sb(es, "pos_f", (128, S_), F32)
        pn = sb(es, "pos_n", (128, S_), F32)
        b1, b2, b3 = TB(), TB(), TB()
        S.dma("sp", pi_[:], A["pos"].partition_broadcast(128), writes=[b1], key="const")
        cp("dve", pf[:], pi_[:], [b1], [b2])
        ts("dve", pf[:], pf[:], tab[:, 0:1], tab[:, 1:2], ALU.mult, ALU.add, [b2, cb], [b2])
        cp("dve", pi_[:], pf[:], [b2], [b1])
        cp("dve", pn[:], pi_[:], [b1], [b3])
        tt("dve", pf[:], pf[:], pn[:], ALU.subtract, [b2, b3], [b2])
        ts("dve", pn[:], pf[:], 0.0, None, ALU.is_lt, None, [b2], [b3])
        tt("dve", pf[:], pf[:], pn[:], ALU.add, [b2, b3], [b2])
        act(pn[:], pf[:], AF.Sin, [b2, cb, b3], [b3], scale=tab[:, 2:3], bias=tab[:, 3:4])
        S.dma("sp", A["CS"], pn[:], reads=[b3], writes=[DT["CS"]], key="st_cs")
        S.barrier()
        es.close()

    def lnt(R, Xsrc, xsb, g, sc0, sh0, outT, outb, want32=None):
        xs = []
        for t4 in range(4):
            t0 = g * 512 + t4 * 128
            xt, xb, xk = R["x"].next()
            S.dma("sp", xt[:], Xsrc[t0:t0 + 128, :], reads=[xsb], writes=[xb], key=xk)
            st, stb, _ = R["st"].next()
            S.op("dve", lambda e: e.bn_stats(st[:, 0:6], xt[:, 0:512]), reads=[xb], writes=[stb])
            S.op("dve", lambda e: e.bn_stats(st[:, 6:12], xt[:, 512:1024]), reads=[xb], writes=[stb])
            S.op("dve", lambda e: e.bn_aggr(st[:, 12:14], st[:, 0:12]), reads=[stb], writes=[stb])
            act(st[:, 14:15], st[:, 13:14], AF.Sqrt, [stb], [stb], bias=LN_EPS, scale=1.0)
            S.op("dve", lambda e: e.reciprocal(st[:, 14:15], st[:, 14:15]), reads=[stb], writes=[stb])
            xn, xnb, _ = R["xn"].next()
            ts("dve", xn[:], xt[:], st[:, 12:13], st[:, 14:15], ALU.subtract, ALU.mult, [xb, stb], [xnb])
            pt, ptb, _ = R["ptr"].next()
            for c in range(8):
                tr(pt[:, c * 128:(c + 1) * 128], xn[:, c * 128:(c + 1) * 128], identb[:], [xnb, cb], [ptb])
            for c in range(8):
                if t4 % 2 == 0:
                    act(outT[:, c, t4 * 128:(t4 + 1) * 128], pt[:, c * 128:(c + 1) * 128], AF.Identity,
                        [ptb, bmodc], [outb], scale=modc[:, sc0 + c:sc0 + c + 1], bias=modc[:, sh0 + c:sh0 + c + 1])
                else:
                    ts("dve", outT[:, c, t4 * 128:(t4 + 1) * 128], pt[:, c * 128:(c + 1) * 128],
                       modc[:, sc0 + c:sc0 + c + 1], modc[:, sh0 + c:sh0 + c + 1], ALU.mult, ALU.add,
                       [ptb, bmodc], [outb])
            if want32 is not None:
                want32(t4, xt, xb, st, stb)
            xs.append((xt, xb))
        return xs

    def resid_ln(R, fsb, fb, xt, xb, lnw, lnb, bcb, dst, dstb, t0, skey):
        stt(fsb[:], xt[:], ALPHA, fsb[:], ALU.mult, ALU.add, [xb, fb], [fb])
        st, stb, _ = R["st"].next()
        S.op("dve", lambda e: e.bn_stats(st[:, 0:6], fsb[:, 0:512]), reads=[fb], writes=[stb])
        S.op("dve", lambda e: e.bn_stats(st[:, 6:12], fsb[:, 512:1024]), reads=[fb], writes=[stb])
        S.op("dve", lambda e: e.bn_aggr(st[:, 12:14], st[:, 0:12]), reads=[stb], writes=[stb])
        act(st[:, 14:15], st[:, 13:14], AF.Sqrt, [stb], [stb], bias=LN_EPS, scale=1.0)
        S.op("dve", lambda e: e.reciprocal(st[:, 14:15], st[:, 14:15]), reads=[stb], writes=[stb])
        ts("dve", fsb[:], fsb[:], st[:, 12:13], st[:, 14:15], ALU.subtract, ALU.mult, [fb, stb], [fb])
        tt("pool", fsb[:], fsb[:], lnw[:], ALU.mult, [fb, bcb], [fb])
        tt("pool", fsb[:], fsb[:], lnb[:], ALU.add, [fb, bcb], [fb])
        S.dma("sp", dst[t0:t0 + 128, :], fsb[:], reads=[fb], writes=[dstb], key=skey)

    def layer_setup(l):
        with ExitStack() as es:
            m48 = sb(es, "m48", (48, 128))
            pT = pst(es, "ls_pT", (128, 48))
            b1, b2 = TB(), PTB()
            S.dma("sp", m48[:], A["MOD"][l].rearrange("(r p) -> r p", p=128), reads=[DT["MOD"]], writes=[b1], key="lc")
            tr(pT[:, 0:48], m48[:, :], identf[0:48, 0:48], [b1, cb], [b2])
            cp("dve", modc[:], pT[:, 0:48], [b2], [bmodc])
            ts("dve", modc[:, 8:16], modc[:, 8:16], 1.0, None, ALU.add, None, [bmodc], [bmodc])
            ts("dve", modc[:, 32:40], modc[:, 32:40], 1.0, None, ALU.add, None, [bmodc], [bmodc])
            S.barrier()

    def stage_m1(l, Xsrc, xsb):
        with ExitStack() as es:
            win = sb(es, "win", (128, 8, DIN), BF16)
            wkr = sb(es, "wkr", (128, 8, 64), BF16)
            wqs = sb(es, "wqs", (128, 3, 8, 128), F32)
            wq = sb(es, "wq", (128, 3, 8, 128), BF16)
            wkvs = sb(es, "wkvs", (128, 2, 8, 128), F32)
            wkv = sb(es, "wkv", (128, 2, 8, 128), BF16)
            ncol = sb(es, "ncol", (128, 8), F32)
            bw, bwq, bwkv, bnc = TB(), TB(), TB(), TB()
            wv = A["w_in"][l].rearrange("(c p) n -> p c n", p=128)
            for c in range(8):
                S.dma("pool", win[:, c, :], wv[:, c, :], writes=[bw], key="w_m1")
            kr0 = SH + 384 + 256
            S.dma("pool", wkr[:, :, 0:32], wv[:, :, kr0:kr0 + 32], writes=[bw], key="w_m1")
            S.dma("pool", wkr[:, :, 32:48], wv[:, :, kr0 + 16:kr0 + 32], writes=[bw], key="w_m1")
            S.dma("pool", wkr[:, :, 48:64], wv[:, :, kr0:kr0 + 16], writes=[bw], key="w_m1")
            uq = A["w_uq"][l].rearrange("(c p) (h d) -> p c h d", p=128, d=96)
            for c in range(3):
                S.dma("sp", wqs[:, c, :, 0:96], uq[:, c, :, :], writes=[bwq], key="w_m1b")
                S.dma("sp", wqs[:, c, :, 96:112], uq[:, c, :, 80:96], writes=[bwq], key="w_m1b")
                S.dma("sp", wqs[:, c, :, 112:128], uq[:, c, :, 64:80], writes=[bwq], key="w_m1b")
            ukv = A["w_ukv"][l].rearrange("(c p) (h d) -> p c h d", p=128, d=128)
            for c in range(2):
                S.dma("sp", wkvs[:, c, :, :], ukv[:, c, :, :], writes=[bwkv], key="w_m1b")
            pTl = pst(es, "m1_pTl", (128, 8))
            bpTl = PTB()
            load_cols(es, A["q_norm_w"][l].rearrange("(r p) -> r p", p=128), 3, ncol[:, 0:3], bnc, pTl, bpTl, "qn")
            load_cols(es, A["kv_norm_w"][l].rearrange("(r p) -> r p", p=128), 2, ncol[:, 4:6], bnc, pTl, bpTl, "kvn")
            for c in range(3):
                ts("dve", wq[:, c, :, :], wqs[:, c, :, :], ncol[:, c:c + 1], None, ALU.mult, None, [bwq, bnc], [bwq])
            for c in range(2):
                ts("dve", wkv[:, c, :, :], wkvs[:, c, :, :], ncol[:, 4 + c:5 + c], None, ALU.mult, None, [bwkv, bnc], [bwkv])
            if "DBG3" in dbg:
                A["DBG3"] = nc.dram_tensor("DBG3", [128, 3, 8, 128], F32, kind="ExternalOutput").ap()
                S.dma("sp", A["DBG3"], wqs[:], reads=[bwq], writes=[TB()], key="dbg")
                A["DBG4"] = nc.dram_tensor("DBG4", [128, 8], F32, kind="ExternalOutput").ap()
                S.dma("sp", A["DBG4"], ncol[:], reads=[bnc], writes=[TB()], key="dbg")
            R = {"x": Ring(es, nc, "m1x", (128, 1024), F32, 2), "st": Ring(es, nc, "m1st", (128, 16), F32, 4),
                 "xn": Ring(es, nc, "m1xn", (128, 1024), BF16, 2),
                 "ptr": Ring(es, nc, "m1ptr", (128, 1024), BF16, 1, psum=True)}
            uTr = Ring(es, nc, "uT", (128, 8, 512), BF16, 2)
            psA = Ring(es, nc, "m1ps", (128, 512), F32, 5, psum=True)
            ps3r = Ring(es, nc, "m1ps3", (128, 8), F32, 1, psum=True)
            stg = Ring(es, nc, "m1stg", (128, SH), F32, 2)
            gst = Ring(es, nc, "m1gst", (128, 512), F32, 3)
            cq = sb(es, "cq", (128, 3, 512), BF16)
            sq = sb(es, "sq", (128, 3, 512), BF16)
            ckv = sb(es, "ckv", (128, 2, 512), BF16)
            sqkv = sb(es, "sqkv", (128, 2, 512), BF16)
            Rq = sb(es, "Rq", (128, 512), F32)
            Rkv = sb(es, "Rkv", (128, 512), F32)
            t1 = Ring(es, nc, "m1t1", (128, 512), F32, 2)
            t2 = Ring(es, nc, "m1t2", (128, 512), F32, 2)
            qtr = Ring(es, nc, "m1qt", (128, 512), BF16, 3)
            ktr = Ring(es, nc, "m1kt", (64, 512), BF16, 3)
            krt = Ring(es, nc, "m1kr", (32, 512), BF16, 2)
            var = Ring(es, nc, "m1va", (128, 8, 128), BF16, 2)
            rkr = Ring(es, nc, "m1rk", (128, 2), F32, 2)
            bcq, bckv, bRq, bRkv = TB(), TB(), TB(), TB()
            for (vt, vb, _) in var.t:
                S.op("dve", lambda e: e.memset(vt[:, :, 64:128], 1.0), writes=[vb])
            csr = Ring(es, nc, "m1cs", (128, 512), F32, 2)
            for g in range(NG):
                gs = slice(g * 512, (g + 1) * 512)
                cs, csb, csk = csr.next()
                S.dma("sp", cs[:], A["CS"][:, gs], reads=[DT["CS"]], writes=[csb], key=csk)
                uT, ub, _ = uTr.next()
                lnt(R, Xsrc, xsb, g, 8, 0, uT, ub)
                for t4 in range(4):
                    sg, sgb, _ = stg.next()
                    for ci, (c0, cw) in enumerate([(0, 512), (512, 512), (1024, 512), (1536, 256)]):
                        ps, pb, _ = psA.next()
                        for c in range(8):
                            mm(ps[:, 0:cw], uT[:, c, t4 * 128:(t4 + 1) * 128], win[:, c, c0:c0 + cw], c == 0, c == 7,
                               [ub, bw], [pb])
                        cp("act" if ci % 2 == 0 else "dve", sg[:, c0:c0 + cw], ps[:, 0:cw], [pb], [sgb])
                    t0 = g * 512 + t4 * 128
                    S.dma("sp", A["PSH"][t0:t0 + 128, :], sg[:], reads=[sgb], writes=[DT["PSH"]], key="st_psh")
                for fc in range(16):
                    ps, pb, _ = psA.next()
                    c0 = SH + 672 + fc * 128
                    for c in range(8):
                        mm(ps[:, :], win[:, c, c0:c0 + 128], uT[:, c, :], c == 0, c == 7, [ub, bw], [pb])
                    gt, gb, _ = gst.next()
                    act(gt[:], ps[:], AF.Sigmoid, [pb], [gb])
                    S.dma("sp", A["GT"][fc * 128:(fc + 1) * 128, gs], gt[:], reads=[gb], writes=[DT["GT"]], key="st_gt")
                for cc in range(3):
                    ps, pb, _ = psA.next()
                    c0 = SH + cc * 128
                    for c in range(8):
                        mm(ps[:, :], win[:, c, c0:c0 + 128], uT[:, c, :], c == 0, c == 7, [ub, bw], [pb])
                    cp("dve", cq[:, cc, :], ps[:], [pb], [bcq])
                    act(sq[:, cc, :], ps[:], AF.Square, [pb], [bcq])
                ps, pb, _ = psA.next()
                for cc in range(3):
                    mm(ps[:, :], onesb[:, :], sq[:, cc, :], cc == 0, cc == 2, [bcq, cb], [pb])
                act(Rq[:], ps[:], AF.Sqrt, [pb], [bRq], scale=96.0 / 384.0, bias=RMS_EPS * 96.0)
                S.op("dve", lambda e: e.reciprocal(Rq[:], Rq[:]), reads=[bRq], writes=[bRq])
                for h in range(8):
                    ps, pb, _ = psA.next()
                    for cc in range(3):
                        mm(ps[:, :], wq[:, cc, h, :], cq[:, cc, :], cc == 0, cc == 2, [bcq, bwq], [pb])
                    qt, qb, _ = qtr.next()
                    a1, a1b, _ = t1.next()
                    a2, a2b, _ = t2.next()
                    tt("dve", qt[0:64, :], ps[0:64, :], Rq[0:64, :], ALU.mult, [pb, bRq], [qb])
                    tt("dve", a1[64:96, :], ps[64:96, :], cs[64:96, :], ALU.mult, [pb, csb], [a1b])
                    tt("dve", a2[64:96, :], ps[96:128, :], cs[96:128, :], ALU.mult, [pb, csb], [a2b])
                    tt("pool", a1[64:96, :], a1[64:96, :], a2[64:96, :], ALU.add, [a1b, a2b], [a1b])
                    tt("pool", qt[64:96, :], a1[64:96, :], Rq[64:96, :], ALU.mult, [a1b, bRq], [qb])
                    S.dma("sp", A["QT"][h, :, gs], qt[0:96, :], reads=[qb], writes=[DT["QT"]], key="st_qt")
                for cc in range(2):
                    ps, pb, _ = psA.next()
                    c0 = SH + 384 + cc * 128
                    for c in range(8):
                        mm(ps[:, :], win[:, c, c0:c0 + 128], uT[:, c, :], c == 0, c == 7, [ub, bw], [pb])
                    cp("dve", ckv[:, cc, :], ps[:], [pb], [bckv])
                    act(sqkv[:, cc, :], ps[:], AF.Square, [pb], [bckv])
                ps, pb, _ = psA.next()
                for cc in range(2):
                    mm(ps[:, :], onesb[:, :], sqkv[:, cc, :], cc == 0, cc == 1, [bckv, cb], [pb])
                act(Rkv[:], ps[:], AF.Sqrt, [pb], [bRkv], scale=1.0 / 256.0, bias=RMS_EPS)
                S.op("dve", lambda e: e.reciprocal(Rkv[:], Rkv[:]), reads=[bRkv], writes=[bRkv])
                ps, pb, _ = psA.next()
                for c in range(8):
                    mm(ps[0:64, :], wkr[:, c, :], uT[:, c, :], c == 0, c == 7, [ub, bw], [pb])
                a1, a1b, _ = t1.next()
                a2, a2b, _ = t2.next()
                kr, krb, _ = krt.next()
                tt("dve", a1[0:32, :], ps[0:32, :], cs[0:32, :], ALU.mult, [pb, csb], [a1b])
                tt("dve", a2[0:32, :], ps[32:64, :], cs[32:64, :], ALU.mult, [pb, csb], [a2b])
                tt("pool", kr[0:32, :], a1[0:32, :], a2[0:32, :], ALU.add, [a1b, a2b], [krb])
                for h in range(8):
                    S.dma("sp", A["KT"][h, 64:96, gs], kr[0:32, :], reads=[krb], writes=[DT["KT"]], key="st_kt")
                for h in range(8):
                    ps, pb, _ = psA.next()
                    for cc in range(2):
                        mm(ps[0:64, :], wkv[:, cc, h, 0:64], ckv[:, cc, :], cc == 0, cc == 1, [bckv, bwkv], [pb])
                    kt, kb, _ = ktr.next()
                    tt("dve", kt[0:64, :], ps[0:64, :], Rkv[0:64, :], ALU.mult, [pb, bRkv], [kb])
                    S.dma("sp", A["KT"][h, 0:64, gs], kt[0:64, :], reads=[kb], writes=[DT["KT"]], key="st_kt")
                for t4 in range(4):
                    tsl = slice(t4 * 128, (t4 + 1) * 128)
                    ps, pb, _ = psA.next()
                    for cc in range(2):
                        mm(ps[:, :], ckv[:, cc, tsl], wkv[:, cc, :, 64:128], cc == 0, cc == 1, [bckv, bwkv], [pb])
                    p3, p3b, _ = ps3r.next()
                    for cc in range(2):
                        mm(p3[:, 0:1], sqkv[:, cc, tsl], onesb[:, 0:1], cc == 0, cc == 1, [bckv, cb], [p3b])
                    rk, rkb, _ = rkr.next()
                    act(rk[:, 0:1], p3[:, 0:1], AF.Sqrt, [p3b], [rkb], scale=1.0 / 256.0, bias=RMS_EPS)
                    S.op("dve", lambda e: e.reciprocal(rk[:, 0:1], rk[:, 0:1]), reads=[rkb], writes=[rkb])
                    va, vb, _ = var.next()
                    act(va[:, :, 0:64], ps[:, :].rearrange("p (h d) -> p h d", d=64), AF.Copy, [pb, rkb], [vb],
                        scale=rk[:, 0:1])
                    t0 = g * 512 + t4 * 128
                    S.dma("sp", A["VA"].rearrange("h t d -> t h d")[t0:t0 + 128, :, :], va[:], reads=[vb],
                          writes=[DT["VA"]], key="st_va")
            S.barrier()

    def stage_m2(l):
        with ExitStack() as es:
            dup = sb(es, "dup", (64, 512))
            aup = sb(es, "aup", (64, 512))
            gup = sb(es, "gup", (128, 512))
            mu = sb(es, "mu", (128, SH))
            bcn = ["decay_w0", "aaa_a0", "k_k", "k_a", "r_k", "gn_w", "gn_b"]
            bc = {n: sb(es, "bc_" + n, (128, 512)) for n in bcn}
            bwb = TB()
            S.dma("sp", dup[:], A["decay_up"][l], writes=[bwb], key="w_m2")
            S.dma("sp", aup[:], A["aaa_up"][l], writes=[bwb], key="w_m2")
            S.dma("sp", gup[:], A["gate_up"][l], writes=[bwb], key="w_m2")
            S.dma("sp", mu[:], A["shift_mu"][l].partition_broadcast(128), writes=[bwb], key="w_m2")
            for n in bcn:
                S.dma("sp", bc[n][:], A[n][l].partition_broadcast(128), writes=[bwb], key="w_m2")
            ST = sb(es, "ST", (64, 8, 64))
            stb = [TB() for _ in range(8)]
            S.op("dve", lambda e: e.memset(ST[:], 0.0), writes=stb)
            pr_ = Ring(es, nc, "m2p", (128, SH), F32, 2)
            pv_ = Ring(es, nc, "m2pv", (128, SH), F32, 2)
            names = ["lw", "lwT", "ld", "a", "gsb", "kk", "tmp", "ss", "km", "b", "bon", "eL", "eLm", "enL", "eD",
                     "Bp", "Kp", "Q4", "QT4", "ysb", "yc", "PCs"]
            shapes = {"lw": (128, 256), "lwT": (128, 384), "ss": (128, 24), "Q4": (128, 4, 512),
                      "QT4": (64, 8, 4, 128), "PCs": (64, 8)}
            T_ = {}
            B_ = {}
            for n in names:
                T_[n] = sb(es, "m2_" + n, shapes.get(n, (128, 512)))
                B_[n] = TB(n)
            yob = sb(es, "m2_yob", (128, 512), F32)
            byob = TB()
            yatr = Ring(es, nc, "m2yat", (128, 4, 128), BF16, 2)
            bk = []
            for i in range(8):
                t_ = es.enter_context(nc.psum_tensor(_uniq("m2bk%d" % i), [128, 512], F32))
                bk.append((t_[:, 0:512], PTB("m2bk%d" % i)))
            G_, A_, B_k, C_, AKB, SM, YA, T4 = range(8)

            def v3(i, a, b):
                return bk[i][0][:, 0:a * b].rearrange("p (a b) -> p a b", b=b)

            class _BR:
                def __init__(self, items):
                    self.items = items
                    self.i = 0

                def next(self):
                    r = self.items[self.i % len(self.items)]
                    self.i += 1
                    return r

            psG = _BR([(PT(bk[i][0], 128), bk[i][1], None) for i in (G_, A_, B_k, C_)])
            pT4r = _BR([(PT(v3(i, 4, 128), 64), bk[i][1], None) for i in (T4, AKB, SM)])
            pakr = _BR([(PT(bk[i][0], 128), bk[i][1], None) for i in (AKB, SM)])
            yall = PT(bk[YA][0], 128)
            byall = bk[YA][1]
            AB4r = Ring(es, nc, "m2AB4", (128, 4, 256), F32, 2)
            AK4r = Ring(es, nc, "m2AK4", (128, 4, 256), F32, 2)
            Y4r = Ring(es, nc, "m2Y4", (128, 4, 128), F32, 3)
            Z4r = Ring(es, nc, "m2Z4", (128, 4, 128), F32, 2)
            TT4r = Ring(es, nc, "m2TT4", (128, 4, 128), F32, 3)
            W4r = Ring(es, nc, "m2W4", (128, 4, 64), F32, 2)
            U4r = Ring(es, nc, "m2U4", (128, 4, 64), F32, 2)
            mN4 = CT["mN"][:].unsqueeze(1).to_broadcast([128, 4, 128])
            id4 = identf[:].unsqueeze(1).to_broadcast([128, 4, 128])

            NEGE = -math.exp(-0.5)
            import os as _o2
            M2STOP = int(_o2.environ.get("MK_M2", "0"))
            try:
                def m2_loads(ti):
                    t0_ = ti * 128
                    p_, pb_2, pk = pr_.next()
                    pv_2, pvb_2, pvk = pv_.next()
                    S.dma("sp", p_[:], A["PSH"][t0_:t0_ + 128, :], reads=[DT["PSH"]], writes=[pb_2], key=pk)
                    if ti == 0:
                        S.op("pool", lambda e: e.memset(pv_2[0:1, :], 0.0), writes=[pvb_2])
                        S.dma("sp", pv_2[1:128, :], A["PSH"][0:127, :], reads=[DT["PSH"]], writes=[pvb_2], key=pvk)
                    else:
                        S.dma("sp", pv_2[:], A["PSH"][t0_ - 1:t0_ + 127, :], reads=[DT["PSH"]], writes=[pvb_2], key=pvk)
                    return p_, pb_2, pv_2, pvb_2

                nxt_ld = m2_loads(0)
                for tI in range(NT):
                    t0 = tI * 128
                    p, pb, pv, pvb = nxt_ld
                    if tI + 1 < NT:
                        nxt_ld = m2_loads(tI + 1)
                    tt("dve", pv[:], pv[:], p[:], ALU.subtract, [pb, pvb], [pvb])
                    tt("dve", pv[:], pv[:], mu[:], ALU.mult, [pvb, bwb], [pvb])
                    tt("dve", p[:], p[:], pv[:], ALU.add, [pb, pvb], [pb])
                    r = p[:, 0:512]
                    k = p[:, 512:1024]
                    v = p[:, 1024:1536]
                    lw, lwT = T_["lw"], T_["lwT"]
                    act(lw[:, 0:64], p[:, 1536:1600], AF.Tanh, [pb], [B_["lw"]])
                    cp("act", lw[:, 64:128], p[:, 1600:1664], [pb], [B_["lw"]])
                    act(lw[:, 128:256], p[:, 1664:1792], AF.Sigmoid, [pb], [B_["lw"]])
                    ps, psb, _ = psG.next()
                    tr(ps[0:64, 0:128], lw[:, 0:64], identf[:], [B_["lw"], cb], [psb])
                    tr(ps[0:64, 128:256], lw[:, 64:128], identf[:], [B_["lw"], cb], [psb])
                    tr(ps[0:128, 256:384], lw[:, 128:256], identf[:], [B_["lw"], cb], [psb])
                    cp("dve", lwT[0:64, 0:256], ps[0:64, 0:256], [psb], [B_["lwT"]])
                    cp("act", lwT[:, 256:384], ps[:, 256:384], [psb], [B_["lwT"]])
                    ps, psb, _ = psG.next()
                    mm(ps[:, :], lwT[0:64, 0:128], dup[:, :], True, True, [B_["lwT"], bwb], [psb])
                    ld = T_["ld"]
                    tt("dve", ld[:], ps[:], bc["decay_w0"][:], ALU.add, [psb, bwb], [B_["ld"]])
                    act(ld[:], ld[:], AF.Sigmoid, [B_["ld"]], [B_["ld"]])
                    ts("dve", ld[:], ld[:], NEGE, None, ALU.mult, None, [B_["ld"]], [B_["ld"]])
                    ps, psb, _ = psG.next()
                    mm(ps[:, :], lwT[0:64, 128:256], aup[:, :], True, True, [B_["lwT"], bwb], [psb])
                    a = T_["a"]
                    tt("dve", a[:], ps[:], bc["aaa_a0"][:], ALU.add, [psb, bwb], [B_["a"]])
                    act(a[:], a[:], AF.Sigmoid, [B_["a"]], [B_["a"]])
                    ps, psb, _ = psG.next()
                    mm(ps[:, :], lwT[:, 256:384], gup[:, :], True, True, [B_["lwT"], bwb], [psb])
                    cp("act", T_["gsb"][:], ps[:], [psb], [B_["gsb"]])
                    kk, tmp, ss = T_["kk"], T_["tmp"], T_["ss"]
                    tt("dve", kk[:], k, bc["k_k"][:], ALU.mult, [pb, bwb], [B_["kk"]])
                    tt("dve", tmp[:], kk[:], kk[:], ALU.mult, [B_["kk"]], [B_["tmp"]])
                    S.op("dve", lambda e: e.tensor_reduce(ss[:, 0:8], tmp[:].rearrange("p (h d) -> p h d", d=64), AX.X, ALU.add),
                         reads=[B_["tmp"]], writes=[B_["ss"]])
                    ts("dve", ss[:, 0:8], ss[:, 0:8], 1e-24, None, ALU.max, None, [B_["ss"]], [B_["ss"]])
                    act(ss[:, 0:8], ss[:, 0:8], AF.Sqrt, [B_["ss"]], [B_["ss"]])
                    S.op("dve", lambda e: e.reciprocal(ss[:, 0:8], ss[:, 0:8]), reads=[B_["ss"]], writes=[B_["ss"]])
                    for h_ in range(8):
                        ts("dve", kk[:, h_ * 64:(h_ + 1) * 64], kk[:, h_ * 64:(h_ + 1) * 64], ss[:, h_:h_ + 1], None,
                           ALU.mult, None, [B_["kk"], B_["ss"]], [B_["kk"]])
                    km, b_, bon = T_["km"], T_["b"], T_["bon"]
                    stt(km[:], a[:], -1.0, bc["k_a"][:], ALU.add, ALU.mult, [B_["a"], bwb], [B_["km"]])
                    stt(km[:], km[:], 1.0, k, ALU.add, ALU.mult, [B_["km"], pb], [B_["km"]])
                    tt("dve", b_[:], kk[:], a[:], ALU.mult, [B_["kk"], B_["a"]], [B_["b"]])
                    tt("dve", tmp[:], r, km[:], ALU.mult, [pb, B_["km"], B_["ss"]], [B_["tmp"]])
                    tt("dve", tmp[:], tmp[:], bc["r_k"][:], ALU.mult, [B_["tmp"], bwb], [B_["tmp"]])
                    S.op("dve", lambda e: e.tensor_reduce(ss[:, 8:16], tmp[:].rearrange("p (h d) -> p h d", d=64), AX.X, ALU.add),
                         reads=[B_["tmp"]], writes=[B_["ss"]])
                    for h_ in range(8):
                        ts("dve", bon[:, h_ * 64:(h_ + 1) * 64], p[:, 1024 + h_ * 64:1024 + (h_ + 1) * 64], ss[:, 8 + h_:9 + h_],
                           None, ALU.mult, None, [pb, B_["ss"]], [B_["bon"]])
                    eL, eLm, enL, eD = T_["eL"], T_["eLm"], T_["enL"], T_["eD"]
                    ps, psb, _ = psG.next()
                    mm(ps[:, :], CT["triinc"][:, :], ld[:, :], True, True, [B_["ld"], cb], [psb])
                    act(eL[:], ps[:], AF.Exp, [psb], [B_["eL"]])
                    act(enL[:], ps[:], AF.Exp, [psb], [B_["enL"]], scale=-1.0)
                    tt("dve", eLm[:], ps[:], ld[:], ALU.subtract, [psb, B_["ld"]], [B_["eLm"]])
                    act(eLm[:], eLm[:], AF.Exp, [B_["eLm"]], [B_["eLm"]])
                    ps, psb, _ = psG.next()
                    mm(ps[:, :], CT["trisu"][:, :], ld[:, :], True, True, [B_["ld"], cb], [psb])
                    act(eD[:], ps[:], AF.Exp, [psb], [B_["eD"]])
                    ps, psb, _ = psG.next()
                    for h in range(8):
                        mm(ps[0:64, h:h + 1], ld[:, h * 64:(h + 1) * 64], onesf[:, 0:1], True, True, [B_["ld"], cb], [psb])
                    act(T_["PCs"][:, :], ps[0:64, 0:8], AF.Exp, [psb], [B_["PCs"]])
                    Q4, QT4 = T_["Q4"], T_["QT4"]
                    tt("dve", Q4[:, 0, :], kk[:], eLm[:], ALU.mult, [B_["kk"], B_["eLm"]], [B_["Q4"]])
                    tt("dve", Q4[:, 1, :], r, eL[:], ALU.mult, [pb, B_["eL"]], [B_["Q4"]])
                    tt("dve", Q4[:, 2, :], b_[:], enL[:], ALU.mult, [B_["b"], B_["enL"]], [B_["Q4"]])
                    tt("dve", Q4[:, 3, :], km[:], enL[:], ALU.mult, [B_["km"], B_["enL"]], [B_["Q4"]])
                    tt("dve", T_["Bp"][:], b_[:], eD[:], ALU.mult, [B_["b"], B_["eD"]], [B_["Bp"]])
                    tt("dve", T_["Kp"][:], km[:], eD[:], ALU.mult, [B_["km"], B_["eD"]], [B_["Kp"]])
                    for h in range(8):
                        p4, p4b, _ = pT4r.next()
                        for q in range(4):
                            tr(p4[0:64, q, :], Q4[:, q, h * 64:(h + 1) * 64], identf[:], [B_["Q4"], cb], [p4b])
                        cp("act" if h % 2 == 0 else "dve", QT4[:, h, :, :], p4[:, :, :], [p4b], [B_["QT4"]])
                    bq = B_["QT4"]
                    A3, Ab = v3(A_, 4, 128), bk[A_][1]
                    B3, Bb = v3(B_k, 4, 128), bk[B_k][1]
                    C3, Cb = v3(C_, 4, 128), bk[C_][1]
                    SM3, SMb = v3(SM, 8, 64), bk[SM][1]
                    for hg in range(2):
                        hh = [4 * hg + j for j in range(4)]
                        AB4, AB4b, _ = AB4r.next()
                        AK4, AK4b, _ = AK4r.next()
                        for j, h in enumerate(hh):
                            kr2 = QT4[:, h, 0:2, :]
                            pak, pakb, _ = pakr.next()
                            mm(pak[:, 0:256], QT4[:, h, 2, :], kr2, True, True, [bq], [pakb])
                            mm(pak[:, 256:512], QT4[:, h, 3, :], kr2, True, True, [bq], [pakb])
                            tt("dve", AB4[:, j, :], pak[:, 0:256], CT["mAB"][:], ALU.mult, [pakb, cb], [AB4b])
                            tt("dve", AK4[:, j, :], pak[:, 256:512], CT["mAK"][:], ALU.mult, [pakb, cb], [AK4b])
                        for j, h in enumerate(hh):
                            mm(C3[:, j, :], QT4[:, h, 0, :], QT4[:, h, 2, :], True, True, [bq], [Cb])
                        Y4, Y4b, _ = Y4r.next()
                        tt("dve", Y4[:], C3, mN4, ALU.mult, [Cb, cb], [Y4b])
                        TT4, TT4b, _ = TT4r.next()
                        tt("dve", TT4[:], AB4[:, :, 0:128], id4, ALU.add, [AB4b, cb], [TT4b])
                        Zt, Zb, zab = AB4, AB4b, True
                        for lv in range(6):
                            def zv(j):
                                return Zt[:, j, 0:128] if zab else Zt[:, j, :]
                            if lv < 5:
                                for j in range(4):
                                    mm(A3[:, j, :], Y4[:, j, :], zv(j), True, True, [Y4b, Zb], [Ab])
                            for j in range(4):
                                mm(B3[:, j, :], zv(j), Y4[:, j, :], True, True, [Y4b, Zb], [Bb])
                            Yn4, Yn4b, _ = Y4r.next()
                            cp("act", Yn4[:], B3, [Bb], [Yn4b])
                            if lv < 5:
                                Zn4, Zn4b, _ = Z4r.next()
                                cp("dve", Zn4[:], A3, [Ab], [Zn4b])
                            for j in range(4):
                                mm(C3[:, j, :], Yn4[:, j, :], TT4[:, j, :], True, True, [Yn4b, TT4b], [Cb])
                            TTn4, TTn4b, _ = TT4r.next()
                            tt("dve", TTn4[:], C3, TT4[:], ALU.add, [Cb, TT4b], [TTn4b])
                            TT4, TT4b = TTn4, TTn4b
                            Y4, Y4b = Yn4, Yn4b
                            if lv < 5:
                                Zt, Zb, zab = Zn4, Zn4b, False
                        for j, h in enumerate(hh):
                            hs = slice(h * 64, (h + 1) * 64)
                            mm(SM3[:, j, :], QT4[:, h, 0, :], ST[:, h, :], True, False, [bq, stb[h]], [SMb])
                            mm(SM3[:, j, :], AK4[:, j, 0:128], v[:, hs], False, True, [AK4b, pb], [SMb])
                        W4, W4b, _ = W4r.next()
                        cp("act", W4[:], SM3[:, 0:4, :], [SMb], [W4b])
                        for j, h in enumerate(hh):
                            mm(SM3[:, 4 + j, :], TT4[:, j, :], W4[:, j, :], True, True, [TT4b, W4b], [SMb])
                        U4, U4b, _ = U4r.next()
                        act(U4[:], SM3[:, 4:8, :], AF.Copy, [SMb], [U4b], scale=-1.0)
                        for j, h in enumerate(hh):
                            hs = slice(h * 64, (h + 1) * 64)
                            mm(yall[:, hs], QT4[:, h, 1, :], ST[:, h, :], True, False, [bq, stb[h]], [byall])
                            mm(yall[:, hs], AB4[:, j, 128:256], U4[:, j, :], False, False, [AB4b, U4b], [byall])
                            mm(yall[:, hs], AK4[:, j, 128:256], v[:, hs], False, True, [AK4b, pb], [byall])
                        for j, h in enumerate(hh):
                            hs = slice(h * 64, (h + 1) * 64)
                            mm(SM3[0:64, j, :], T_["Bp"][:, hs], U4[:, j, :], True, False, [B_["Bp"], U4b], [SMb])
                            mm(SM3[0:64, j, :], T_["Kp"][:, hs], v[:, hs], False, True, [B_["Kp"], pb], [SMb])
                        for j, h in enumerate(hh):
                            stt(ST[:, h, :], ST[:, h, :], T_["PCs"][:, h:h + 1], SM3[0:64, j, :], ALU.mult, ALU.add,
                                [stb[h], B_["PCs"], SMb], [stb[h]])
                    ysb, yc = T_["ysb"], T_["yc"]
                    cp("act", ysb[:], yall[:], [byall], [B_["ysb"]])
                    ysb3 = ysb[:].rearrange("p (h d) -> p h d", d=64)
                    yc3 = yc[:].rearrange("p (h d) -> p h d", d=64)
                    S.op("dve", lambda e: e.tensor_reduce(ss[:, 16:24], ysb3, AX.X, ALU.add), reads=[B_["ysb"]], writes=[B_["ss"]])
                    ts("dve", ss[:, 16:24], ss[:, 16:24], 1.0 / 64.0, None, ALU.mult, None, [B_["ss"]], [B_["ss"]])
                    for h_ in range(8):
                        ts("dve", yc[:, h_ * 64:(h_ + 1) * 64], ysb[:, h_ * 64:(h_ + 1) * 64], ss[:, 16 + h_:17 + h_], None,
                           ALU.subtract, None, [B_["ysb"], B_["ss"]], [B_["yc"]])
                    tt("dve", tmp[:], yc[:], yc[:], ALU.mult, [B_["yc"]], [B_["tmp"]])
                    S.op("dve", lambda e: e.tensor_reduce(ss[:, 16:24], tmp[:].rearrange("p (h d) -> p h d", d=64), AX.X, ALU.add),
                         reads=[B_["tmp"], B_["yc"]], writes=[B_["ss"]])
                    act(ss[:, 16:24], ss[:, 16:24], AF.Sqrt, [B_["ss"]], [B_["ss"]], scale=1.0 / 64.0, bias=GN_EPS)
                    S.op("dve", lambda e: e.reciprocal(ss[:, 16:24], ss[:, 16:24]), reads=[B_["ss"]], writes=[B_["ss"]])
                    for h_ in range(8):
                        ts("dve", yc[:, h_ * 64:(h_ + 1) * 64], yc[:, h_ * 64:(h_ + 1) * 64], ss[:, 16 + h_:17 + h_], None,
                           ALU.mult, None, [B_["yc"], B_["ss"]], [B_["yc"]])
                    tt("dve", yc[:], yc[:], bc["gn_w"][:], ALU.mult, [B_["yc"], bwb], [B_["yc"]])
                    tt("dve", yc[:], yc[:], bc["gn_b"][:], ALU.add, [B_["yc"], bwb], [B_["yc"]])
                    tt("dve", yc[:], yc[:], bon[:], ALU.add, [B_["yc"], B_["bon"]], [B_["yc"]])
                    tt("dve", yob[:], yc[:], T_["gsb"][:], ALU.mult, [B_["yc"], B_["gsb"]], [byob])
                    pb4, pb4b = v3(T4, 4, 128), bk[T4][1]
                    for j in range(4):
                        tr(pb4[:, j, :], yob[:, j * 128:(j + 1) * 128], identf[:], [byob, cb], [pb4b])
                    yat, yatb, _ = yatr.next()
                    cp("act", yat[:], pb4, [pb4b], [yatb])
                    S.dma("sp", A["YAT"].rearrange("(c p) t -> p c t", p=128)[:, :, t0:t0 + 128], yat[:], reads=[yatb],
                          writes=[DT["YAT"]], key="st_yat")
            except _StopM2:
                pass
            S.barrier()

    def stage_m3(l):
        with ExitStack() as es:
            qr = Ring(es, nc, "m3q", (96, S_), BF16, 2)
            kr_ = Ring(es, nc, "m3k", (96, S_), BF16, 2)
            vr = Ring(es, nc, "m3v", (128, NT, 128), BF16, 2)
            psS = Ring(es, nc, "m3ps", (128, 512), F32, 4, psum=True)
            psO = Ring(es, nc, "m3po", (128, 512), F32, 2, psum=True)
            ptr_ = Ring(es, nc, "m3pt", (128, 512), BF16, 3)
            rcr = Ring(es, nc, "m3rc", (64, 512), F32, 2)
            ybr = Ring(es, nc, "m3yb", (64, 512), BF16, 2)
            def m3_loads(hh_):
                qT_, qb_, qk = qr.next()
                kT_, kb_, kk_ = kr_.next()
                va_, vb_, vk = vr.next()
                S.dma("sp", qT_[:], A["QT"][hh_], reads=[DT["QT"]], writes=[qb_], key=qk)
                S.dma("sp", kT_[:], A["KT"][hh_], reads=[DT["KT"]], writes=[kb_], key=kk_)
                S.dma("sp", va_[:], A["VA"][hh_].rearrange("(n p) d -> p n d", p=128), reads=[DT["VA"]], writes=[vb_], key=vk)
                return qT_, qb_, kT_, kb_, va_, vb_

            nxt3 = m3_loads(0)
            for h in range(8):
                qT, qb, kT, kb, va, vb = nxt3
                if h + 1 < 8:
                    nxt3 = m3_loads(h + 1)
                for g in range(NG):
                    gs = slice(g * 512, (g + 1) * 512)
                    po, pob, _ = psO.next()
                    nj = 4 * g + 4

                    def m3_scores(j):
                        ps_, psb_, _ = psS.next()
                        diag = j >= 4 * g
                        mm(ps_[:, :], kT[0:96, j * 128:(j + 1) * 128], qT[0:96, gs], True, not diag, [kb, qb], [psb_])
                        if diag:
                            mm(ps_[:, :], identb[:, :], CT["cmask"][:, j - 4 * g, :], False, True, [cb], [psb_])
                        return ps_, psb_

                    cur_s = m3_scores(0)
                    for j in range(nj):
                        ps, psb = cur_s
                        if j + 1 < nj:
                            cur_s = m3_scores(j + 1)
                        pt, ptb, _ = ptr_.next()
                        act(pt[:], ps[:], AF.Exp, [psb], [ptb])
                        mm(po[:, :], va[:, j, :], pt[:], j == 0, j == nj - 1, [vb, ptb], [pob])
                    rc, rcb, _ = rcr.next()
                    yb, ybb, _ = ybr.next()
                    S.op("dve", lambda e: e.reciprocal(rc[0:64, :], po[64:128, :]), reads=[pob], writes=[rcb])
                    tt("dve", yb[0:64, :], po[0:64, :], rc[0:64, :], ALU.mult, [pob, rcb], [ybb])
                    S.dma("sp", A["YBT"][h * 64:(h + 1) * 64, gs], yb[:], reads=[ybb], writes=[DT["YBT"]], key="st_ybt")
            S.barrier()

    def stage_m4(l, Xsrc, xsb, Xdst, xdb):
        with ExitStack() as es:
            pa = sb(es, "pa", (128, 4, 1024), BF16)
            pb_ = sb(es, "pb", (128, 4, 1024), BF16)
            wo = sb(es, "wo", (128, 8, 1024), BF16)
            g1 = sb(es, "g1bc", (128, 1024))
            lnw = sb(es, "lnw", (128, 1024))
            lnb = sb(es, "lnb", (128, 1024))
            bw, bcb = TB(), TB()
            S.dma("pool", pa[:], A["p_a"][l].rearrange("(c p) n -> p c n", p=128), writes=[bw], key="w_m4")
            S.dma("pool", pb_[:], A["p_b"][l].rearrange("(c p) n -> p c n", p=128), writes=[bw], key="w_m4")
            S.dma("pool", wo[:], A["w_o"][l].rearrange("(c p) n -> p c n", p=128), writes=[bw], key="w_m4")
            S.dma("sp", g1[:], A["MOD"][l, 2048:3072].partition_broadcast(128), reads=[DT["MOD"]], writes=[bcb], key="w_m4b")
            S.dma("sp", lnw[:], A["ln1_w"][l].partition_broadcast(128), writes=[bcb], key="w_m4b")
            S.dma("sp", lnb[:], A["ln1_b"][l].partition_broadcast(128), writes=[bcb], key="w_m4b")
            R = {"st": Ring(es, nc, "m4st", (128, 16), F32, 4)}
            yar = Ring(es, nc, "m4ya", (128, 4, 512), BF16, 2)
            ybr = Ring(es, nc, "m4yb", (128, 4, 512), BF16, 2)
            gar = Ring(es, nc, "m4ga", (128, 512), F32, 3)
            gbr = Ring(es, nc, "m4gb", (128, 512), F32, 3)
            t1r = Ring(es, nc, "m4t1", (128, 512), F32, 2)
            t2r = Ring(es, nc, "m4t2", (128, 512), F32, 2)
            mTr = Ring(es, nc, "m4mT", (128, 8, 512), BF16, 2)
            xr = Ring(es, nc, "m4x", (128, 1024), F32, 3)
            fr = Ring(es, nc, "m4f", (128, 1024), F32, 3)
            psA = Ring(es, nc, "m4ps", (128, 512), F32, 6, psum=True)
            yav = A["YAT"].rearrange("(c p) t -> p c t", p=128)
            ybv = A["YBT"].rearrange("(c p) t -> p c t", p=128)
            for g in range(NG):
                gs = slice(g * 512, (g + 1) * 512)
                ya, yab, yak = yar.next()
                yb, ybb, ybk = ybr.next()
                S.dma("sp", ya[:], yav[:, :, gs], reads=[DT["YAT"]], writes=[yab], key=yak)
                S.dma("sp", yb[:], ybv[:, :, gs], reads=[DT["YBT"]], writes=[ybb], key=ybk)
                mT, mTb, _ = mTr.next()
                for fc in range(8):
                    fs = slice(fc * 128, (fc + 1) * 128)
                    ga, gab, gak = gar.next()
                    gb, gbb, gbk = gbr.next()
                    S.dma("sp", ga[:], A["GT"][fc * 128:(fc + 1) * 128, gs], reads=[DT["GT"]], writes=[gab], key=gak)
                    S.dma("sp", gb[:], A["GT"][1024 + fc * 128:1024 + (fc + 1) * 128, gs], reads=[DT["GT"]], writes=[gbb], key=gbk)
                    p1, p1b, _ = psA.next()
                    for c in range(4):
                        mm(p1[:, :], pa[:, c, fs], ya[:, c, :], c == 0, c == 3, [bw, yab], [p1b])
                    p2, p2b, _ = psA.next()
                    for c in range(4):
                        mm(p2[:, :], pb_[:, c, fs], yb[:, c, :], c == 0, c == 3, [bw, ybb], [p2b])
                    a1, a1b, _ = t1r.next()
                    a2, a2b, _ = t2r.next()
                    tt("dve", a1[:], p1[:], ga[:], ALU.mult, [p1b, gab], [a1b])
                    tt("dve", a2[:], p2[:], gb[:], ALU.mult, [p2b, gbb], [a2b])
                    tt("pool", mT[:, fc, :], a1[:], a2[:], ALU.add, [a1b, a2b], [mTb])
                for t4 in range(4):
                    t0 = g * 512 + t4 * 128
                    xt, xb, xk = xr.next()
                    S.dma("sp", xt[:], Xsrc[t0:t0 + 128, :], reads=[xsb], writes=[xb], key=xk)
                    f, fb, _ = fr.next()
                    for cg in range(2):
                        pm, pmb, _ = psA.next()
                        for c in range(8):
                            mm(pm[:, :], mT[:, c, t4 * 128:(t4 + 1) * 128], wo[:, c, cg * 512:(cg + 1) * 512], c == 0, c == 7,
                               [mTb, bw], [pmb])
                        tt("dve", f[:, cg * 512:(cg + 1) * 512], pm[:], g1[:, cg * 512:(cg + 1) * 512], ALU.mult,
                           [pmb, bcb], [fb])
                    resid_ln(R, f, fb, xt, xb, lnw, lnb, bcb, Xdst, xdb, t0, "st_x")
            S.barrier()

    def stage_ffn(l, Xsrc, xsb, Xdst, xdb, moe):
        F_ = DFFE if moe else DFF
        FB = 512 if moe else 256
        nch = FB // 128
        nblk = F_ // FB
        nexp = NE if moe else 1
        li = l // 2
        with ExitStack() as es:
            g2 = sb(es, "g2bc", (128, 1024))
            lnw = sb(es, "lnw2", (128, 1024))
            lnb = sb(es, "lnb2", (128, 1024))
            bcb = TB()
            S.dma("sp", g2[:], A["MOD"][l, 5120:6144].partition_broadcast(128), reads=[DT["MOD"]], writes=[bcb], key="w_f")
            S.dma("sp", lnw[:], A["ln2_w"][l].partition_broadcast(128), writes=[bcb], key="w_f")
            S.dma("sp", lnb[:], A["ln2_b"][l].partition_broadcast(128), writes=[bcb], key="w_f")
            R = {"x": Ring(es, nc, "fx", (128, 1024), F32, 6), "st": Ring(es, nc, "fst", (128, 16), F32, 4),
                 "xn": Ring(es, nc, "fxn", (128, 1024), BF16, 2),
                 "ptr": Ring(es, nc, "fptr", (128, 1024), BF16, 1, psum=True)}
            hTr = Ring(es, nc, "hT", (128, 8, 512), BF16, 2)
            wgr = Ring(es, nc, "wg" if moe else "wgd", (128, 8, FB), BF16, 2)
            wur = Ring(es, nc, "wu" if moe else "wud", (128, 8, FB), BF16, 2)
            wdr = Ring(es, nc, "wd" if moe else "wdd", (128, nch, 1024), BF16, 2)
            if not moe:
                for nm, src in (("WG", "ffn_w_gate"), ("WU", "ffn_w_up"), ("WD", "ffn_w_down")):
                    S.dma("pool", A[nm].rearrange("(p r) n -> p (r n)", p=128),
                          A[src][li].rearrange("(p r) n -> p (r n)", p=128), writes=[DT[nm]], key="pre")
            psG = Ring(es, nc, "fpsG", (128, 512), F32, 2, psum=True)
            psU = Ring(es, nc, "fpsU", (128, 512), F32, 2, psum=True)
            psD = Ring(es, nc, "fpsD", (128, 512), F32, 2 if moe else 3, psum=True)
            sgr = Ring(es, nc, "fsg", (128, 512), F32, 2)
            aTr = Ring(es, nc, "faT", (128, nch, 512), BF16, 2)
            accr = Ring(es, nc, "facc", (128, 1024), F32, 8)
            if moe:
                rw = sb(es, "rw", (128, 8, 8))
                rbb = sb(es, "rbb", (128, 8))
                S.dma("sp", rw[:], A["router_w"][li].rearrange("(c p) e -> p c e", p=128), writes=[bcb], key="w_f")
                S.dma("sp", rbb[:], A["router_b"][li].partition_broadcast(128), writes=[bcb], key="w_f")
                x32r = Ring(es, nc, "fx32", (128, 1024), F32, 2)
                h32r = Ring(es, nc, "fh32", (128, 8, 128), F32, 2)
                p32r = Ring(es, nc, "fp32", (128, 512), F32, 1, psum=True)
                lgr = Ring(es, nc, "flg", (128, 48), F32, 8)
            for g in range(NG):
                hT, hb, _ = hTr.next()
                gws = []

                def router(t4, xt, xb, st, stb):
                    x32, x32b, _ = x32r.next()
                    ts("dve", x32[:], xt[:], st[:, 12:13], st[:, 14:15], ALU.subtract, ALU.mult, [xb, stb], [x32b])
                    h32, h32b, _ = h32r.next()
                    for half in range(2):
                        pp, ppb, _ = p32r.next()
                        for c4 in range(4):
                            c = half * 4 + c4
                            tr(pp[:, c4 * 128:(c4 + 1) * 128], x32[:, c * 128:(c + 1) * 128], identf[:], [x32b, cb], [ppb])
                        for c4 in range(4):
                            c = half * 4 + c4
                            act(h32[:, c, :], pp[:, c4 * 128:(c4 + 1) * 128], AF.Identity, [ppb, bmodc], [h32b],
                                scale=modc[:, 32 + c:33 + c], bias=modc[:, 24 + c:25 + c])
                    pp, ppb, _ = p32r.next()
                    for c in range(8):
                        mm(pp[:, 0:8], h32[:, c, :], rw[:, c, :], c == 0, c == 7, [h32b, bcb], [ppb])
                    lg, lgb, _ = lgr.next()
                    L_ = lg[:, 0:8]
                    tt("dve", L_, pp[:, 0:8], rbb[:], ALU.add, [ppb, bcb], [lgb])
                    S.op("dve", lambda e: e.tensor_reduce(lg[:, 40:41], L_, AX.X, ALU.max), reads=[lgb], writes=[lgb])
                    ts("dve", lg[:, 8:16], L_, lg[:, 40:41], None, ALU.is_equal, None, [lgb], [lgb])
                    stt(lg[:, 16:24], lg[:, 8:16], -1e30, L_, ALU.mult, ALU.add, [lgb], [lgb])
                    S.op("dve", lambda e: e.tensor_reduce(lg[:, 41:42], lg[:, 16:24], AX.X, ALU.max), reads=[lgb], writes=[lgb])
                    ts("dve", lg[:, 24:32], lg[:, 16:24], lg[:, 41:42], None, ALU.is_equal, None, [lgb], [lgb])
                    tt("dve", lg[:, 42:43], lg[:, 40:41], lg[:, 41:42], ALU.subtract, [lgb], [lgb])
                    act(lg[:, 43:44], lg[:, 42:43], AF.Sigmoid, [lgb], [lgb])
                    act(lg[:, 44:45], lg[:, 42:43], AF.Sigmoid, [lgb], [lgb], scale=-1.0)
                    ts("dve", lg[:, 32:40], lg[:, 8:16], lg[:, 43:44], None, ALU.mult, None, [lgb], [lgb])
                    stt(lg[:, 32:40], lg[:, 24:32], lg[:, 44:45], lg[:, 32:40], ALU.mult, ALU.add, [lgb], [lgb])
                    gws.append((lg, lgb))

                xs = lnt(R, Xsrc, xsb, g, 32, 24, hT, hb, want32=router if moe else None)
                accs = [accr.next() for _ in range(4)]
                first = True
                for e_ in range(nexp):
                    if moe:
                        wgv = A["moe_w_gate"][li, e_].rearrange("(c p) n -> p c n", p=128)
                        wuv = A["moe_w_up"][li, e_].rearrange("(c p) n -> p c n", p=128)
                        wdv = A["moe_w_down"][li, e_].rearrange("(c p) n -> p c n", p=128)
                    else:
                        wgv = A["WG"].rearrange("(c p) n -> p c n", p=128)
                        wuv = A["WU"].rearrange("(c p) n -> p c n", p=128)
                        wdv = A["WD"].rearrange("(c p) n -> p c n", p=128)
                    for bi in range(nblk):
                        wg, wgb, wgk = wgr.next()
                        wu, wub, wuk = wur.next()
                        wd, wdb, wdk = wdr.next()
                        wq_ = "pool" if moe else "sp"
                        wrd = [] if moe else [DT["WG"], DT["WU"], DT["WD"]]
                        S.dma(wq_, wg[:], wgv[:, :, bi * FB:(bi + 1) * FB], reads=wrd[0:1], writes=[wgb], key=wgk)
                        S.dma(wq_, wu[:], wuv[:, :, bi * FB:(bi + 1) * FB], reads=wrd[1:2], writes=[wub], key=wuk)
                        S.dma(wq_, wd[:], wdv[:, bi * nch:(bi + 1) * nch, :], reads=wrd[2:3], writes=[wdb], key=wdk)
                        aT, aTb, _ = aTr.next()
                        for fc in range(nch):
                            pg, pgb, _ = psG.next()
                            for c in range(8):
                                mm(pg[:, :], wg[:, c, fc * 128:(fc + 1) * 128], hT[:, c, :], c == 0, c == 7, [wgb, hb], [pgb])
                            pu, pub, _ = psU.next()
                            for c in range(8):
                                mm(pu[:, :], wu[:, c, fc * 128:(fc + 1) * 128], hT[:, c, :], c == 0, c == 7, [wub, hb], [pub])
                            sg, sgb, _ = sgr.next()
                            act(sg[:], pg[:], AF.Silu, [pgb], [sgb])
                            tt("dve", aT[:, fc, :], sg[:], pu[:], ALU.mult, [sgb, pub], [aTb])
                        for t4 in range(4):
                            acc, accb, _ = accs[t4]
                            for cg in range(2):
                                pd, pdb, _ = psD.next()
                                for fc in range(nch):
                                    mm(pd[:, :], aT[:, fc, t4 * 128:(t4 + 1) * 128], wd[:, fc, cg * 512:(cg + 1) * 512],
                                       fc == 0, fc == nch - 1, [aTb, wdb], [pdb])
                                cs_ = slice(cg * 512, (cg + 1) * 512)
                                if moe:
                                    lg, lgb = gws[t4]
                                    if first:
                                        ts("dve", acc[:, cs_], pd[:], lg[:, 32 + e_:33 + e_], None, ALU.mult, None,
                                           [pdb, lgb], [accb])
                                    else:
                                        stt(acc[:, cs_], pd[:], lg[:, 32 + e_:33 + e_], acc[:, cs_], ALU.mult, ALU.add,
                                            [pdb, lgb, accb], [accb])
                                else:
                                    if first:
                                        cp("act", acc[:, cs_], pd[:], [pdb], [accb])
                                    else:
                                        tt("dve", acc[:, cs_], pd[:], acc[:, cs_], ALU.add, [pdb, accb], [accb])
                        first = False
                for t4 in range(4):
                    t0 = g * 512 + t4 * 128
                    acc, accb, _ = accs[t4]
                    xt, xb = xs[t4]
                    tt("pool", acc[:], acc[:], g2[:], ALU.mult, [accb, bcb], [accb])
                    resid_ln(R, acc, accb, xt, xb, lnw, lnb, bcb, Xdst, xdb, t0, "st_x")
            S.barrier()

    import os
    STOP = os.environ.get("MK_STOP", "")
    cur, curb = A["x"], DT["x"]
    for l in range(NL):
        if STOP == "ada":
            break
        layer_setup(l)
        if STOP == "setup":
            break
        stage_m1(l, cur, curb)
        if STOP == "m1":
            break
        stage_m2(l)
        if STOP == "m2":
            break
        stage_m3(l)
        if STOP == "m3":
            break
        stage_m4(l, cur, curb, A["XB"], DT["XB"])
        if STOP == "m4":
            break
        last = (l == NL - 1)
        dst, dstb = (Y, DT["y"]) if last else (A["XA"], DT["XA"])
        stage_ffn(l, A["XB"], DT["XB"], dst, dstb, moe=(l % 2 == 1))
        cur, curb = A["XA"], DT["XA"]
    S.barrier()
    G.close()
    nsem = len(S.sems)
    S.close()
    return nc, nsem, S.nins


_CACHE = {}


def run(inputs, S_, NL, ncores, dbg=()):
    key = (S_, NL, tuple(dbg))
    if key not in _CACHE:
        _CACHE[key] = build(S_, NL, dbg)
    nc, nsem, nins = _CACHE[key]
    consts = host_consts()
    shared = {}
    for n, s in WSHAPES:
        a = np.ascontiguousarray(inputs[n], dtype=np.float32)
        shared[n] = a.reshape(s)
    shared.update(consts)
    in_maps = []
    for b in range(ncores):
        m = dict(shared)
        m["x"] = np.ascontiguousarray(inputs["x"][b], dtype=np.float32)
        m["c"] = np.ascontiguousarray(inputs["c"][b], dtype=np.float32).reshape(8, 128)
        m["pos"] = np.ascontiguousarray(inputs["positions"][b], dtype=np.int32)
        in_maps.append(m)
    res = run_bass_kernel_spmd(nc, in_maps, core_ids=list(range(ncores)))
    return res


def kernel(**inputs):
    inputs = {k: np.asarray(v) for k, v in inputs.items()}
    B, S_, _ = inputs["x"].shape
    res = run(inputs, S_, 4, B)
    return np.stack([res.results[b]["y"] for b in range(B)], axis=0).astype(np.float32)
```

```python
import math
from contextlib import ExitStack
import numpy as np
import concourse.bass as bass
import concourse.mybir as mybir
from concourse.bass_utils import run_bass_kernel_spmd

F32 = mybir.dt.float32
BF16 = mybir.dt.bfloat16
I32 = mybir.dt.int32
AF = mybir.ActivationFunctionType
ALU = mybir.AluOpType
AX = mybir.AxisListType

D = 1024
SH = 1792
DIN = 4512
ALPHA = 8.0 ** 0.25
LN_EPS = 1e-5
RMS_EPS = 1e-6
GN_EPS = 64e-5
DFF = 2816
DFFE = 3584
NE = 8

WSHAPES = [
    ("w_ada", (4, 1024, 6144)), ("b_ada", (4, 6144)), ("w_in", (4, 1024, 4512)), ("shift_mu", (4, 1792)),
    ("decay_w0", (4, 512)), ("decay_up", (4, 64, 512)), ("aaa_a0", (4, 512)), ("aaa_up", (4, 64, 512)),
    ("gate_up", (4, 128, 512)), ("k_k", (4, 512)), ("k_a", (4, 512)), ("r_k", (4, 512)), ("gn_w", (4, 512)),
    ("gn_b", (4, 512)), ("q_norm_w", (4, 384)), ("w_uq", (4, 384, 768)), ("kv_norm_w", (4, 256)),
    ("w_ukv", (4, 256, 1024)), ("p_a", (4, 512, 1024)), ("p_b", (4, 512, 1024)), ("w_o", (4, 1024, 1024)),
    ("ln1_w", (4, 1024)), ("ln1_b", (4, 1024)), ("ln2_w", (4, 1024)), ("ln2_b", (4, 1024)),
    ("ffn_w_gate", (2, 1024, 2816)), ("ffn_w_up", (2, 1024, 2816)), ("ffn_w_down", (2, 2816, 1024)),
    ("router_w", (2, 1024, 8)), ("router_b", (2, 8)),
    ("moe_w_gate", (2, 8, 1024, 3584)), ("moe_w_up", (2, 8, 1024, 3584)), ("moe_w_down", (2, 8, 3584, 1024)),
]


class _StopM2(Exception):
    pass


class TB:
    __slots__ = ("name", "w", "r", "psum")

    def __init__(self, name="", psum=False):
        self.name = name
        self.w = None
        self.r = {}
        self.psum = psum


def PTB(name=""):
    return TB(name, psum=True)


class Sync:
    def __init__(self, nc):
        self.nc = nc
        self.eng = {"pe": nc.tensor, "act": nc.scalar, "dve": nc.vector, "pool": nc.gpsimd, "sp": nc.sync}
        self.sems = {}
        self.count = {}
        self.known = {e: {} for e in self.eng}
        self.ctx = []
        self.nins = 0
        self.rotc = {}

    def sem(self, key):
        if key not in self.sems:
            cm = self.nc.semaphore("s_" + key.replace("#", "_"))
            self.sems[key] = cm.__enter__()
            self.ctx.append(cm)
            self.count[key] = 0
        return self.sems[key]

    def _wait(self, e, need):
        kn = self.known[e]
        for k, v in need.items():
            if kn.get(k, 0) < v:
                self.eng[e].wait_ge(self.sem(k), v)
                kn[k] = v

    def _need(self, e, reads, writes, pe_acc=False):
        need = {}

        def add(ev):
            if ev is None:
                return
            k, v = ev
            if need.get(k, 0) < v:
                need[k] = v
        own = "c_" + e
        for b in reads:
            add(b.w)
            if b.psum:
                for k, v in b.r.items():
                    if k != own:
                        add((k, v))
        for b in writes:
            if not (pe_acc and b.w is not None and b.w[0] == "c_pe"):
                add(b.w)
            for k, v in b.r.items():
                add((k, v))
        self._wait(e, need)

    def _mark(self, ev, reads, writes):
        k, v = ev
        for b in reads:
            if b.r.get(k, 0) < v:
                b.r[k] = v
        for b in writes:
            b.w = ev
            b.r = {}

    def op(self, e, fn, reads=(), writes=(), pe_acc=False):
        self._need(e, reads, writes, pe_acc)
        ins = fn(self.eng[e])
        key = "c_" + e
        s = self.sem(key)
        self.count[key] += 1
        ins.then_inc(s, 1)
        self.nins += 1
        self._mark((key, self.count[key]), reads, writes)

    ROT = {"pre": 3, "const": 3, "ada": 2, "w_m1": 2, "w_m1b": 2, "w_m2": 2, "w_m4": 2, "w_m4b": 2, "w_f": 2,
           "st_psh": 2, "st_gt": 2, "st_qt": 2, "st_kt": 2, "st_va": 2, "st_yat": 2, "st_ybt": 2, "st_x": 2}

    def dma(self, q, out, in_, reads=(), writes=(), key="x", **kw):
        self._need(q, reads, writes)
        rot = self.ROT.get(key, 1)
        key = "d_" + key
        if rot > 1:
            i = self.rotc.get(key, 0)
            self.rotc[key] = i + 1
            key = "%s#%d" % (key, i % rot)
        s = self.sem(key)
        self._wait(q, {key: self.count[key]})
        ins = self.eng[q].dma_start(out=out, in_=in_, **kw)
        self.count[key] += 16
        ins.then_inc(s, 16)
        self.nins += 1
        self._mark((key, self.count[key]), reads, writes)

    def barrier(self, engines=("pe", "act", "dve", "pool", "sp")):
        need = {k: v for k, v in self.count.items() if v > 0}
        for e in engines:
            self._wait(e, need)

    def close(self):
        for cm in reversed(self.ctx):
            cm.__exit__(None, None, None)
        self.ctx = []


class PT:
    def __init__(self, t, p):
        self.t = t
        self.p = p

    def __getitem__(self, key):
        if not isinstance(key, tuple):
            key = (key,)
        k0 = key[0]
        if isinstance(k0, slice) and k0.start is None and k0.stop is None:
            k0 = slice(0, self.p)
        return self.t[(k0,) + tuple(key[1:])]


_UID = [0]


def _uniq(name):
    _UID[0] += 1
    return "%s_u%d" % (name, _UID[0])


def _psum_bank(es, nc, name, shape, dt):
    isz = 4 if dt == F32 else 2
    nfree = 1
    for d in shape[1:]:
        nfree *= d
    assert nfree * isz <= 2048, (name, shape)
    t = es.enter_context(nc.psum_tensor(name, [128, 2048 // isz], dt))
    v = t[:, 0:nfree]
    if len(shape) == 3:
        v = v.rearrange("p (a b) -> p a b", b=shape[2])
    return PT(v, shape[0])


class Ring:
    def __init__(self, es, nc, name, shape, dt, n, psum=False):
        self.t = []
        base = name
        name = _uniq(name)
        for i in range(n):
            if psum:
                t = _psum_bank(es, nc, "%s_%d" % (name, i), shape, dt)
            else:
                t = PT(es.enter_context(nc.sbuf_tensor("%s_%d" % (name, i), [128] + list(shape[1:]), dt)), shape[0])
            self.t.append((t, TB("%s_%d" % (name, i), psum=psum), "%s_%d" % (base, i)))
        self.i = 0

    def next(self):
        r = self.t[self.i % len(self.t)]
        self.i += 1
        return r


def host_consts():
    j = np.arange(128)
    c = {}
    c["identf"] = np.eye(128, dtype=np.float32)
    c["triinc"] = (j[:, None] <= j[None, :]).astype(np.float32)
    c["trisu"] = (j[:, None] > j[None, :]).astype(np.float32)
    strict = (j[:, None] < j[None, :]).astype(np.float32)
    incl = (j[:, None] <= j[None, :]).astype(np.float32)
    c["mAB"] = np.concatenate([-strict, incl], axis=1)
    c["mAK"] = np.concatenate([strict, incl], axis=1)
    c["mN"] = -(j[:, None] > j[None, :]).astype(np.float32)
    q = np.arange(512)
    cm = np.zeros((128, 4, 512), np.float32)
    for jj in range(4):
        cm[:, jj, :] = np.where((jj * 128 + j[:, None]) > q[None, :], -30000.0, 0.0)
    c["cmask"] = cm
    invf = np.exp(-math.log(10000.0) * np.arange(16, dtype=np.float32) / 16).astype(np.float32)
    tab = np.zeros((128, 4), np.float32)
    for p in range(128):
        qq = p % 64
        i = qq % 16
        tab[p, 0] = invf[i] / (2 * math.pi)
        if qq < 32:
            tab[p, 1:] = (0.75, 2 * math.pi, -math.pi)
        elif qq < 48:
            tab[p, 1:] = (0.5, -2 * math.pi, math.pi)
        else:
            tab[p, 1:] = (0.5, 2 * math.pi, -math.pi)
    c["ropetab"] = tab
    return c


CONST_SHAPES = [("identf", (128, 128)), ("triinc", (128, 128)), ("trisu", (128, 128)), ("mAB", (128, 256)),
                ("mAK", (128, 256)), ("mN", (128, 128)), ("cmask", (128, 4, 512)), ("ropetab", (128, 4))]


def build(S_, NL, dbg=()):
    nc = bass.Bass("TRN2", target_bir_lowering=False)
    NT, NG = S_ // 128, S_ // 512
    A = {}

    def din(name, shape, dt=F32):
        A[name] = nc.dram_tensor(name, list(shape), dt, kind="ExternalInput").ap()

    din("x", (S_, D))
    din("c", (8, 128))
    din("pos", (S_,), I32)
    for n, s in WSHAPES:
        din(n, s)
    for n, s in CONST_SHAPES:
        din(n, s)
    Y = nc.dram_tensor("y", [S_, D], F32, kind="ExternalOutput").ap()
    DT = {}

    def scr(name, shape, dt):
        kind = "ExternalOutput" if name in dbg else "Internal"
        A[name] = nc.dram_tensor(name, list(shape), dt, kind=kind).ap()
        DT[name] = TB(name)

    scr("MOD", (NL, 6144), F32)
    scr("XA", (S_, D), F32)
    scr("XB", (S_, D), F32)
    scr("PSH", (S_, SH), F32)
    scr("QT", (8, 96, S_), BF16)
    scr("KT", (8, 96, S_), BF16)
    scr("VA", (8, S_, 128), BF16)
    scr("GT", (2048, S_), F32)
    scr("YAT", (512, S_), BF16)
    scr("YBT", (512, S_), BF16)
    scr("CS", (128, S_), F32)
    scr("WG", (1024, DFF), BF16)
    scr("WU", (1024, DFF), BF16)
    scr("WD", (DFF, 1024), BF16)
    DT["x"] = TB("x")
    DT["y"] = TB("y")

    S = Sync(nc)
    G = ExitStack()

    def sb(es, name, shape, dt=F32):
        return PT(es.enter_context(nc.sbuf_tensor(_uniq(name), [128] + list(shape[1:]), dt)), shape[0])

    def pst(es, name, shape, dt=F32):
        return _psum_bank(es, nc, _uniq(name), shape, dt)

    def mm(out, lhsT, rhs, start, stop, reads, writes):
        S.op("pe", lambda e: e.matmul(out, lhsT, rhs, start=start, stop=stop), reads=reads, writes=writes, pe_acc=True)

    def tr(out, in_, ident, reads, writes):
        S.op("pe", lambda e: e.transpose(out, in_, ident), reads=reads, writes=writes, pe_acc=True)

    def act(out, in_, func, reads, writes, **kw):
        S.op("act", lambda e: e.activation(out, in_, func, **kw), reads=reads, writes=writes)

    def tt(eng, out, in0, in1, op, reads, writes):
        S.op(eng, lambda e: e.tensor_tensor(out, in0, in1, op), reads=reads, writes=writes)

    def ts(eng, out, in0, s1, s2, op0, op1, reads, writes):
        if op1 is None:
            S.op(eng, lambda e: e.tensor_scalar(out, in0, s1, None, op0), reads=reads, writes=writes)
        else:
            S.op(eng, lambda e: e.tensor_scalar(out, in0, s1, s2, op0, op1), reads=reads, writes=writes)

    def stt(out, in0, sc, in1, op0, op1, reads, writes):
        S.op("dve", lambda e: e.scalar_tensor_tensor(out, in0, sc, in1, op0, op1), reads=reads, writes=writes)

    def cp(eng, out, in_, reads, writes):
        if eng == "act":
            act(out, in_, AF.Copy, reads, writes)
        else:
            S.op(eng, lambda e: e.tensor_copy(out, in_), reads=reads, writes=writes)

    CT = {}
    cb = TB("consts")
    for n, s in CONST_SHAPES:
        if n == "cmask":
            CT[n] = sb(G, "c_" + n, s, BF16)
            S.dma("pool", CT[n][:], A[n], writes=[cb], key="constp")
        else:
            CT[n] = sb(G, "c_" + n, s, F32)
            S.dma("sp", CT[n][:], A[n], writes=[cb], key="const")
    identf = CT["identf"]
    identb = sb(G, "identb", (128, 128), BF16)
    onesf = sb(G, "onesf", (128, 128), F32)
    onesb = sb(G, "onesb", (128, 128), BF16)
    modc = sb(G, "modc", (128, 48), F32)
    bmodc = TB("modc")
    cp("dve", identb[:], identf[:], [cb], [cb])
    S.op("dve", lambda e: e.memset(onesf[:], 1.0), writes=[cb])
    S.op("dve", lambda e: e.memset(onesb[:], 1.0), writes=[cb])
    tab = CT["ropetab"]
    def load_cols(es_, vec2d, n, dst, dstb, pT_, pTb, tag):
        tmp = sb(es_, "lc_" + tag, (n, 128), F32)
        tb_ = TB()
        S.dma("sp", tmp[:], vec2d, writes=[tb_], key="lc")
        tr(pT_[:, 0:n], tmp[:, :], identf[0:n, 0:n], [tb_, cb], [pTb])
        cp("dve", dst, pT_[:, 0:n], [pTb], [dstb])

    with ExitStack() as es:
        c8 = sb(es, "c8", (8, 128))
        cT = sb(es, "cT", (128, 8))
        modrow = sb(es, "modrow", (1, 6144))
        brow = sb(es, "brow", (1, 6144))
        wr = Ring(es, nc, "wada", (128, 8, 512), F32, 2)
        pT = pst(es, "ada_pT", (128, 8))
        pr = Ring(es, nc, "ada_ps", (1, 512), F32, 2, psum=True)
        bc8, bcT, bmr, bbr, bpT = TB(), TB(), TB(), TB(), PTB()
        S.dma("sp", c8[:], A["c"], writes=[bc8], key="ada")
        act(c8[:], c8[:], AF.Silu, [bc8], [bc8])
        tr(pT[:, 0:8], c8[:, :], identf[0:8, 0:8], [bc8, cb], [bpT])
        cp("dve", cT[:], pT[:, 0:8], [bpT], [bcT])
        if "DBG" in dbg:
            A["DBG"] = nc.dram_tensor("DBG", [128, 8], F32, kind="ExternalOutput").ap()
            S.dma("sp", A["DBG"], cT[:], reads=[bcT], writes=[TB()], key="dbg")
        for l in range(NL):
            S.dma("sp", brow[:], A["b_ada"][l:l + 1, :], writes=[bbr], key="ada")
            wv = A["w_ada"][l].rearrange("(c p) n -> p c n", p=128)
            for cg in range(12):
                wt, wb, wk = wr.next()
                S.dma("sp", wt[:], wv[:, :, cg * 512:(cg + 1) * 512], writes=[wb], key=wk)
                if "DBG2" in dbg and cg == 0 and l == 0:
                    A["DBG2"] = nc.dram_tensor("DBG2", [128, 8, 512], F32, kind="ExternalOutput").ap()
                    S.dma("sp", A["DBG2"], wt[:], reads=[wb], writes=[TB()], key="dbg")
                ps, pb, _ = pr.next()
                for c in range(8):
                    mm(ps[0:1, :], cT[:, c:c + 1], wt[:, c, :], c == 0, c == 7, [bcT, wb], [pb])
                tt("dve", modrow[0:1, cg * 512:(cg + 1) * 512], ps[0:1, :], brow[0:1, cg * 512:(cg + 1) * 512],
                   ALU.add, [pb, bbr], [bmr])
            S.dma("sp", A["MOD"][l:l + 1, :], modrow[:], reads=[bmr], writes=[DT["MOD"]], key="st_mod")
        S.barrier()

    import os as _os
    for es in ([] if _os.environ.get("MK_NOROPE") else [ExitStack()]):
        pi_ = sb(es, "pos_i", (128, S_), I32)
        pf = sb(es, "pos_f", (128, S_), F32)
        pn = sb(es, "pos_n", (128, S_), F32)
        b1, b2, b3 = TB(), TB(), TB()
        S.dma("sp", pi_[:], A["pos"].partition_broadcast(128), writes=[b1], key="const")
        cp("dve", pf[:], pi_[:], [b1], [b2])
        ts("dve", pf[:], pf[:], tab[:, 0:1], tab[:, 1:2], ALU.mult, ALU.add, [b2, cb], [b2])
        cp("dve", pi_[:], pf[:], [b2], [b1])
        cp("dve", pn[:], pi_[:], [b1], [b3])
        tt("dve", pf[:], pf[:], pn[:], ALU.subtract, [b2, b3], [b2])
        ts("dve", pn[:], pf[:], 0.0, None, ALU.is_lt, None, [b2], [b3])
        tt("dve", pf[:], pf[:], pn[:], ALU.add, [b2, b3], [b2])
        act(pn[:], pf[:], AF.Sin, [b2, cb, b3], [b3], scale=tab[:, 2:3], bias=tab[:, 3:4])
        S.dma("sp", A["CS"], pn[:], reads=[b3], writes=[DT["CS"]], key="st_cs")
        S.barrier()
        es.close()

    def lnt(R, Xsrc, xsb, g, sc0, sh0, outT, outb, want32=None):
        xs = []
        for t4 in range(4):
            t0 = g * 512 + t4 * 128
            xt, xb, xk = R["x"].next()
            S.dma("sp", xt[:], Xsrc[t0:t0 + 128, :], reads=[xsb], writes=[xb], key=xk)
            st, stb, _ = R["st"].next()
            S.op("dve", lambda e: e.bn_stats(st[:, 0:6], xt[:, 0:512]), reads=[xb], writes=[stb])
            S.op("dve", lambda e: e.bn_stats(st[:, 6:12], xt[:, 512:1024]), reads=[xb], writes=[stb])
            S.op("dve", lambda e: e.bn_aggr(st[:, 12:14], st[:, 0:12]), reads=[stb], writes=[stb])
            act(st[:, 14:15], st[:, 13:14], AF.Sqrt, [stb], [stb], bias=LN_EPS, scale=1.0)
            S.op("dve", lambda e: e.reciprocal(st[:, 14:15], st[:, 14:15]), reads=[stb], writes=[stb])
            xn, xnb, _ = R["xn"].next()
            ts("dve", xn[:], xt[:], st[:, 12:13], st[:, 14:15], ALU.subtract, ALU.mult, [xb, stb], [xnb])
            pt, ptb, _ = R["ptr"].next()
            for c in range(8):
                tr(pt[:, c * 128:(c + 1) * 128], xn[:, c * 128:(c + 1) * 128], identb[:], [xnb, cb], [ptb])
            for c in range(8):
                if t4 % 2 == 0:
                    act(outT[:, c, t4 * 128:(t4 + 1) * 128], pt[:, c * 128:(c + 1) * 128], AF.Identity,
                        [ptb, bmodc], [outb], scale=modc[:, sc0 + c:sc0 + c + 1], bias=modc[:, sh0 + c:sh0 + c + 1])
                else:
                    ts("dve", outT[:, c, t4 * 128:(t4 + 1) * 128], pt[:, c * 128:(c + 1) * 128],
                       modc[:, sc0 + c:sc0 + c + 1], modc[:, sh0 + c:sh0 + c + 1], ALU.mult, ALU.add,
                       [ptb, bmodc], [outb])
            if want32 is not None:
                want32(t4, xt, xb, st, stb)
            xs.append((xt, xb))
        return xs

    def resid_ln(R, fsb, fb, xt, xb, lnw, lnb, bcb, dst, dstb, t0, skey):
        stt(fsb[:], xt[:], ALPHA, fsb[:], ALU.mult, ALU.add, [xb, fb], [fb])
        st, stb, _ = R["st"].next()
        S.op("dve", lambda e: e.bn_stats(st[:, 0:6], fsb[:, 0:512]), reads=[fb], writes=[stb])
        S.op("dve", lambda e: e.bn_stats(st[:, 6:12], fsb[:, 512:1024]), reads=[fb], writes=[stb])
        S.op("dve", lambda e: e.bn_aggr(st[:, 12:14], st[:, 0:12]), reads=[stb], writes=[stb])
        act(st[:, 14:15], st[:, 13:14], AF.Sqrt, [stb], [stb], bias=LN_EPS, scale=1.0)
        S.op("dve", lambda e: e.reciprocal(st[:, 14:15], st[:, 14:15]), reads=[stb], writes=[stb])
        ts("dve", fsb[:], fsb[:], st[:, 12:13], st[:, 14:15], ALU.subtract, ALU.mult, [fb, stb], [fb])
        tt("pool", fsb[:], fsb[:], lnw[:], ALU.mult, [fb, bcb], [fb])
        tt("pool", fsb[:], fsb[:], lnb[:], ALU.add, [fb, bcb], [fb])
        S.dma("sp", dst[t0:t0 + 128, :], fsb[:], reads=[fb], writes=[dstb], key=skey)

    def layer_setup(l):
        with ExitStack() as es:
            m48 = sb(es, "m48", (48, 128))
            pT = pst(es, "ls_pT", (128, 48))
            b1, b2 = TB(), PTB()
            S.dma("sp", m48[:], A["MOD"][l].rearrange("(r p) -> r p", p=128), reads=[DT["MOD"]], writes=[b1], key="lc")
            tr(pT[:, 0:48], m48[:, :], identf[0:48, 0:48], [b1, cb], [b2])
            cp("dve", modc[:], pT[:, 0:48], [b2], [bmodc])
            ts("dve", modc[:, 8:16], modc[:, 8:16], 1.0, None, ALU.add, None, [bmodc], [bmodc])
            ts("dve", modc[:, 32:40], modc[:, 32:40], 1.0, None, ALU.add, None, [bmodc], [bmodc])
            S.barrier()

    def stage_m1(l, Xsrc, xsb):
        with ExitStack() as es:
            win = sb(es, "win", (128, 8, DIN), BF16)
            wkr = sb(es, "wkr", (128, 8, 64), BF16)
            wqs = sb(es, "wqs", (128, 3, 8, 128), F32)
            wq = sb(es, "wq", (128, 3, 8, 128), BF16)
            wkvs = sb(es, "wkvs", (128, 2, 8, 128), F32)
            wkv = sb(es, "wkv", (128, 2, 8, 128), BF16)
            ncol = sb(es, "ncol", (128, 8), F32)
            bw, bwq, bwkv, bnc = TB(), TB(), TB(), TB()
            wv = A["w_in"][l].rearrange("(c p) n -> p c n", p=128)
            for c in range(8):
                S.dma("pool", win[:, c, :], wv[:, c, :], writes=[bw], key="w_m1")
            kr0 = SH + 384 + 256
            S.dma("pool", wkr[:, :, 0:32], wv[:, :, kr0:kr0 + 32], writes=[bw], key="w_m1")
            S.dma("pool", wkr[:, :, 32:48], wv[:, :, kr0 + 16:kr0 + 32], writes=[bw], key="w_m1")
            S.dma("pool", wkr[:, :, 48:64], wv[:, :, kr0:kr0 + 16], writes=[bw], key="w_m1")
            uq = A["w_uq"][l].rearrange("(c p) (h d) -> p c h d", p=128, d=96)
            for c in range(3):
                S.dma("sp", wqs[:, c, :, 0:96], uq[:, c, :, :], writes=[bwq], key="w_m1b")
                S.dma("sp", wqs[:, c, :, 96:112], uq[:, c, :, 80:96], writes=[bwq], key="w_m1b")
                S.dma("sp", wqs[:, c, :, 112:128], uq[:, c, :, 64:80], writes=[bwq], key="w_m1b")
            ukv = A["w_ukv"][l].rearrange("(c p) (h d) -> p c h d", p=128, d=128)
            for c in range(2):
                S.dma("sp", wkvs[:, c, :, :], ukv[:, c, :, :], writes=[bwkv], key="w_m1b")
            pTl = pst(es, "m1_pTl", (128, 8))
            bpTl = PTB()
            load_cols(es, A["q_norm_w"][l].rearrange("(r p) -> r p", p=128), 3, ncol[:, 0:3], bnc, pTl, bpTl, "qn")
            load_cols(es, A["kv_norm_w"][l].rearrange("(r p) -> r p", p=128), 2, ncol[:, 4:6], bnc, pTl, bpTl, "kvn")
            for c in range(3):
                ts("dve", wq[:, c, :, :], wqs[:, c, :, :], ncol[:, c:c + 1], None, ALU.mult, None, [bwq, bnc], [bwq])
            for c in range(2):
                ts("dve", wkv[:, c, :, :], wkvs[:, c, :, :], ncol[:, 4 + c:5 + c], None, ALU.mult, None, [bwkv, bnc], [bwkv])
            if "DBG3" in dbg:
                A["DBG3"] = nc.dram_tensor("DBG3", [128, 3, 8, 128], F32, kind="ExternalOutput").ap()
                S.dma("sp", A["DBG3"], wqs[:], reads=[bwq], writes=[TB()], key="dbg")
                A["DBG4"] = nc.dram_tensor("DBG4", [128, 8], F32, kind="ExternalOutput").ap()
                S.dma("sp", A["DBG4"], ncol[:], reads=[bnc], writes=[TB()], key="dbg")
            R = {"x": Ring(es, nc, "m1x", (128, 1024), F32, 2), "st": Ring(es, nc, "m1st", (128, 16), F32, 4),
                 "xn": Ring(es, nc, "m1xn", (128, 1024), BF16, 2),
                 "ptr": Ring(es, nc, "m1ptr", (128, 1024), BF16, 1, psum=True)}
            uTr = Ring(es, nc, "uT", (128, 8, 512), BF16, 2)
            psA = Ring(es, nc, "m1ps", (128, 512), F32, 5, psum=True)
            ps3r = Ring(es, nc, "m1ps3", (128, 8), F32, 1, psum=True)
            stg = Ring(es, nc, "m1stg", (128, SH), F32, 2)
            gst = Ring(es, nc, "m1gst", (128, 512), F32, 3)
            cq = sb(es, "cq", (128, 3, 512), BF16)
            sq = sb(es, "sq", (128, 3, 512), BF16)
            ckv = sb(es, "ckv", (128, 2, 512), BF16)
            sqkv = sb(es, "sqkv", (128, 2, 512), BF16)
            Rq = sb(es, "Rq", (128, 512), F32)
            Rkv = sb(es, "Rkv", (128, 512), F32)
            t1 = Ring(es, nc, "m1t1", (128, 512), F32, 2)
            t2 = Ring(es, nc, "m1t2", (128, 512), F32, 2)
            qtr = Ring(es, nc, "m1qt", (128, 512), BF16, 3)
            ktr = Ring(es, nc, "m1kt", (64, 512), BF16, 3)
            krt = Ring(es, nc, "m1kr", (32, 512), BF16, 2)
            var = Ring(es, nc, "m1va", (128, 8, 128), BF16, 2)
            rkr = Ring(es, nc, "m1rk", (128, 2), F32, 2)
            bcq, bckv, bRq, bRkv = TB(), TB(), TB(), TB()
            for (vt, vb, _) in var.t:
                S.op("dve", lambda e: e.memset(vt[:, :, 64:128], 1.0), writes=[vb])
            csr = Ring(es, nc, "m1cs", (128, 512), F32, 2)
            for g in range(NG):
                gs = slice(g * 512, (g + 1) * 512)
                cs, csb, csk = csr.next()
                S.dma("sp", cs[:], A["CS"][:, gs], reads=[DT["CS"]], writes=[csb], key=csk)
                uT, ub, _ = uTr.next()
                lnt(R, Xsrc, xsb, g, 8, 0, uT, ub)
                for t4 in range(4):
                    sg, sgb, _ = stg.next()
                    for ci, (c0, cw) in enumerate([(0, 512), (512, 512), (1024, 512), (1536, 256)]):
                        ps, pb, _ = psA.next()
                        for c in range(8):
                            mm(ps[:, 0:cw], uT[:, c, t4 * 128:(t4 + 1) * 128], win[:, c, c0:c0 + cw], c == 0, c == 7,
                               [ub, bw], [pb])
                        cp("act" if ci % 2 == 0 else "dve", sg[:, c0:c0 + cw], ps[:, 0:cw], [pb], [sgb])
                    t0 = g * 512 + t4 * 128
                    S.dma("sp", A["PSH"][t0:t0 + 128, :], sg[:], reads=[sgb], writes=[DT["PSH"]], key="st_psh")
                for fc in range(16):
                    ps, pb, _ = psA.next()
                    c0 = SH + 672 + fc * 128
                    for c in range(8):
                        mm(ps[:, :], win[:, c, c0:c0 + 128], uT[:, c, :], c == 0, c == 7, [ub, bw], [pb])
                    gt, gb, _ = gst.next()
                    act(gt[:], ps[:], AF.Sigmoid, [pb], [gb])
                    S.dma("sp", A["GT"][fc * 128:(fc + 1) * 128, gs], gt[:], reads=[gb], writes=[DT["GT"]], key="st_gt")
                for cc in range(3):
                    ps, pb, _ = psA.next()
                    c0 = SH + cc * 128
                    for c in range(8):
                        mm(ps[:, :], win[:, c, c0:c0 + 128], uT[:, c, :], c == 0, c == 7, [ub, bw], [pb])
                    cp("dve", cq[:, cc, :], ps[:], [pb], [bcq])
                    act(sq[:, cc, :], ps[:], AF.Square, [pb], [bcq])
                ps, pb, _ = psA.next()
                for cc in range(3):
                    mm(ps[:, :], onesb[:, :], sq[:, cc, :], cc == 0, cc == 2, [bcq, cb], [pb])
                act(Rq[:], ps[:], AF.Sqrt, [pb], [bRq], scale=96.0 / 384.0, bias=RMS_EPS * 96.0)
                S.op("dve", lambda e: e.reciprocal(Rq[:], Rq[:]), reads=[bRq], writes=[bRq])
                for h in range(8):
                    ps, pb, _ = psA.next()
                    for cc in range(3):
                        mm(ps[:, :], wq[:, cc, h, :], cq[:, cc, :], cc == 0, cc == 2, [bcq, bwq], [pb])
                    qt, qb, _ = qtr.next()
                    a1, a1b, _ = t1.next()
                    a2, a2b, _ = t2.next()
                    tt("dve", qt[0:64, :], ps[0:64, :], Rq[0:64, :], ALU.mult, [pb, bRq], [qb])
                    tt("dve", a1[64:96, :], ps[64:96, :], cs[64:96, :], ALU.mult, [pb, csb], [a1b])
                    tt("dve", a2[64:96, :], ps[96:128, :], cs[96:128, :], ALU.mult, [pb, csb], [a2b])
                    tt("pool", a1[64:96, :], a1[64:96, :], a2[64:96, :], ALU.add, [a1b, a2b], [a1b])
                    tt("pool", qt[64:96, :], a1[64:96, :], Rq[64:96, :], ALU.mult, [a1b, bRq], [qb])
                    S.dma("sp", A["QT"][h, :, gs], qt[0:96, :], reads=[qb], writes=[DT["QT"]], key="st_qt")
                for cc in range(2):
                    ps, pb, _ = psA.next()
                    c0 = SH + 384 + cc * 128
                    for c in range(8):
                        mm(ps[:, :], win[:, c, c0:c0 + 128], uT[:, c, :], c == 0, c == 7, [ub, bw], [pb])
                    cp("dve", ckv[:, cc, :], ps[:], [pb], [bckv])
                    act(sqkv[:, cc, :], ps[:], AF.Square, [pb], [bckv])
                ps, pb, _ = psA.next()
                for cc in range(2):
                    mm(ps[:, :], onesb[:, :], sqkv[:, cc, :], cc == 0, cc == 1, [bckv, cb], [pb])
                act(Rkv[:], ps[:], AF.Sqrt, [pb], [bRkv], scale=1.0 / 256.0, bias=RMS_EPS)
                S.op("dve", lambda e: e.reciprocal(Rkv[:], Rkv[:]), reads=[bRkv], writes=[bRkv])
                ps, pb, _ = psA.next()
                for c in range(8):
                    mm(ps[0:64, :], wkr[:, c, :], uT[:, c, :], c == 0, c == 7, [ub, bw], [pb])
                a1, a1b, _ = t1.next()
                a2, a2b, _ = t2.next()
                kr, krb, _ = krt.next()
                tt("dve", a1[0:32, :], ps[0:32, :], cs[0:32, :], ALU.mult, [pb, csb], [a1b])
                tt("dve", a2[0:32, :], ps[32:64, :], cs[32:64, :], ALU.mult, [pb, csb], [a2b])
                tt("pool", kr[0:32, :], a1[0:32, :], a2[0:32, :], ALU.add, [a1b, a2b], [krb])
                for h in range(8):
                    S.dma("sp", A["KT"][h, 64:96, gs], kr[0:32, :], reads=[krb], writes=[DT["KT"]], key="st_kt")
                for h in range(8):
                    ps, pb, _ = psA.next()
                    for cc in range(2):
                        mm(ps[0:64, :], wkv[:, cc, h, 0:64], ckv[:, cc, :], cc == 0, cc == 1, [bckv, bwkv], [pb])
                    kt, kb, _ = ktr.next()
                    tt("dve", kt[0:64, :], ps[0:64, :], Rkv[0:64, :], ALU.mult, [pb, bRkv], [kb])
                    S.dma("sp", A["KT"][h, 0:64, gs], kt[0:64, :], reads=[kb], writes=[DT["KT"]], key="st_kt")
                for t4 in range(4):
                    tsl = slice(t4 * 128, (t4 + 1) * 128)
                    ps, pb, _ = psA.next()
                    for cc in range(2):
                        mm(ps[:, :], ckv[:, cc, tsl], wkv[:, cc, :, 64:128], cc == 0, cc == 1, [bckv, bwkv], [pb])
                    p3, p3b, _ = ps3r.next()
                    for cc in range(2):
                        mm(p3[:, 0:1], sqkv[:, cc, tsl], onesb[:, 0:1], cc == 0, cc == 1, [bckv, cb], [p3b])
                    rk, rkb, _ = rkr.next()
                    act(rk[:, 0:1], p3[:, 0:1], AF.Sqrt, [p3b], [rkb], scale=1.0 / 256.0, bias=RMS_EPS)
                    S.op("dve", lambda e: e.reciprocal(rk[:, 0:1], rk[:, 0:1]), reads=[rkb], writes=[rkb])
                    va, vb, _ = var.next()
                    act(va[:, :, 0:64], ps[:, :].rearrange("p (h d) -> p h d", d=64), AF.Copy, [pb, rkb], [vb],
                        scale=rk[:, 0:1])
                    t0 = g * 512 + t4 * 128
                    S.dma("sp", A["VA"].rearrange("h t d -> t h d")[t0:t0 + 128, :, :], va[:], reads=[vb],
                          writes=[DT["VA"]], key="st_va")
            S.barrier()

    def stage_m2(l):
        with ExitStack() as es:
            dup = sb(es, "dup", (64, 512))
            aup = sb(es, "aup", (64, 512))
            gup = sb(es, "gup", (128, 512))
            mu = sb(es, "mu", (128, SH))
            bcn = ["decay_w0", "aaa_a0", "k_k", "k_a", "r_k", "gn_w", "gn_b"]
            bc = {n: sb(es, "bc_" + n, (128, 512)) for n in bcn}
            bwb = TB()
            S.dma("sp", dup[:], A["decay_up"][l], writes=[bwb], key="w_m2")
            S.dma("sp", aup[:], A["aaa_up"][l], writes=[bwb], key="w_m2")
            S.dma("sp", gup[:], A["gate_up"][l], writes=[bwb], key="w_m2")
            S.dma("sp", mu[:], A["shift_mu"][l].partition_broadcast(128), writes=[bwb], key="w_m2")
            for n in bcn:
                S.dma("sp", bc[n][:], A[n][l].partition_broadcast(128), writes=[bwb], key="w_m2")
            ST = sb(es, "ST", (64, 8, 64))
            stb = [TB() for _ in range(8)]
            S.op("dve", lambda e: e.memset(ST[:], 0.0), writes=stb)
            pr_ = Ring(es, nc, "m2p", (128, SH), F32, 2)
            pv_ = Ring(es, nc, "m2pv", (128, SH), F32, 2)
            names = ["lw", "lwT", "ld", "a", "kk", "tmp", "ss", "km", "b", "eL", "eLm", "enL", "eD",
                     "Q4", "ysb", "yc"]
            shapes = {"lw": (128, 256), "lwT": (128, 384), "ss": (128, 24), "Q4": (128, 4, 512),
                      "QT4": (64, 8, 4, 128), "PCs": (64, 8)}
            T_ = {}
            B_ = {}
            for n in names:
                T_[n] = sb(es, "m2_" + n, shapes.get(n, (128, 512)))
                B_[n] = TB(n)
            yob = sb(es, "m2_yob", (128, 512), F32)
            byob = TB()
            yatr = Ring(es, nc, "m2yat", (128, 4, 128), BF16, 2)
            bk = []
            for i in range(8):
                t_ = es.enter_context(nc.psum_tensor(_uniq("m2bk%d" % i), [128, 512], F32))
                bk.append((t_[:, 0:512], PTB("m2bk%d" % i)))
            G_, A_, B_k, C_, AKB, SM, YA, T4 = range(8)

            def v3(i, a, b):
                return bk[i][0][:, 0:a * b].rearrange("p (a b) -> p a b", b=b)

            class _BR:
                def __init__(self, items):
                    self.items = items
                    self.i = 0

                def next(self):
                    r = self.items[self.i % len(self.items)]
                    self.i += 1
                    return r

            psG = _BR([(PT(bk[i][0], 128), bk[i][1], None) for i in (G_, A_, B_k, C_)])
            pT4r = _BR([(PT(v3(i, 4, 128), 64), bk[i][1], None) for i in (T4, AKB, SM)])
            pakr = _BR([(PT(bk[i][0], 128), bk[i][1], None) for i in (AKB, SM)])
            yall = PT(bk[YA][0], 128)
            byall = bk[YA][1]
            AB4r = Ring(es, nc, "m2AB4", (128, 4, 256), F32, 2)
            AK4r = Ring(es, nc, "m2AK4", (128, 4, 256), F32, 2)
            Y4r = Ring(es, nc, "m2Y4", (128, 4, 128), F32, 3)
            Z4r = Ring(es, nc, "m2Z4", (128, 4, 128), F32, 2)
            TT4r = Ring(es, nc, "m2TT4", (128, 4, 128), F32, 3)
            W4r = Ring(es, nc, "m2W4", (128, 4, 64), F32, 2)
            U4r = Ring(es, nc, "m2U4", (128, 4, 64), F32, 2)
            mN4 = CT["mN"][:].unsqueeze(1).to_broadcast([128, 4, 128])
            id4 = identf[:].unsqueeze(1).to_broadcast([128, 4, 128])

            NEGE = -math.exp(-0.5)
            import os as _o2
            M2STOP = int(_o2.environ.get("MK_M2", "0"))
            try:
                def m2_loads(ti):
                    t0_ = ti * 128
                    p_, pb_2, pk = pr_.next()
                    pv_2, pvb_2, pvk = pv_.next()
                    S.dma("sp", p_[:], A["PSH"][t0_:t0_ + 128, :], reads=[DT["PSH"]], writes=[pb_2], key=pk)
                    if ti == 0:
                        S.op("pool", lambda e: e.memset(pv_2[0:1, :], 0.0), writes=[pvb_2])
                        S.dma("sp", pv_2[1:128, :], A["PSH"][0:127, :], reads=[DT["PSH"]], writes=[pvb_2], key=pvk)
                    else:
                        S.dma("sp", pv_2[:], A["PSH"][t0_ - 1:t0_ + 127, :], reads=[DT["PSH"]], writes=[pvb_2], key=pvk)
                    return p_, pb_2, pv_2, pvb_2

                sets = []
                for q_ in range(2):
                    d_ = {n_: sb(es, 'm2q%d_%s' % (q_, n_), shapes.get(n_, (128, 512))) for n_ in ('QT4', 'Bp', 'Kp', 'PCs', 'bon', 'gsb')}
                    sets.append((d_, {n_: TB(n_) for n_ in d_}))
                T_['ssg'] = sb(es, 'm2_ssg', (128, 24)); B_['ssg'] = TB('ssg')
                T_['tmpg'] = sb(es, 'm2_tmpg', (128, 512)); B_['tmpg'] = TB('tmpg')
                prG = _BR([(PT(bk[i][0], 128), bk[i][1], None) for i in (G_, T4)])
                prT = _BR([(PT(v3(i, 4, 128), 64), bk[i][1], None) for i in (T4, G_)])
                lds = {}

                def prep(tI):
                    T = dict(T_)
                    T.update(sets[tI % 2][0])
                    B = dict(B_)
                    B.update(sets[tI % 2][1])
                    p, pb, pv, pvb = lds[tI]
                    tt("dve", pv[:], pv[:], p[:], ALU.subtract, [pb, pvb], [pvb])
                    tt("dve", pv[:], pv[:], mu[:], ALU.mult, [pvb, bwb], [pvb])
                    tt("dve", p[:], p[:], pv[:], ALU.add, [pb, pvb], [pb])
                    r = p[:, 0:512]
                    k = p[:, 512:1024]
                    v = p[:, 1024:1536]
                    lw, lwT = T["lw"], T["lwT"]
                    act(lw[:, 0:64], p[:, 1536:1600], AF.Tanh, [pb], [B["lw"]])
                    cp("act", lw[:, 64:128], p[:, 1600:1664], [pb], [B["lw"]])
                    act(lw[:, 128:256], p[:, 1664:1792], AF.Sigmoid, [pb], [B["lw"]])
                    yield
                    ps, psb, _ = prG.next()
                    tr(ps[0:64, 0:128], lw[:, 0:64], identf[:], [B["lw"], cb], [psb])
                    tr(ps[0:64, 128:256], lw[:, 64:128], identf[:], [B["lw"], cb], [psb])
                    tr(ps[0:128, 256:384], lw[:, 128:256], identf[:], [B["lw"], cb], [psb])
                    cp("dve", lwT[0:64, 0:256], ps[0:64, 0:256], [psb], [B["lwT"]])
                    cp("act", lwT[:, 256:384], ps[:, 256:384], [psb], [B["lwT"]])
                    yield
                    ps, psb, _ = prG.next()
                    mm(ps[:, :], lwT[0:64, 0:128], dup[:, :], True, True, [B["lwT"], bwb], [psb])
                    ld = T["ld"]
                    tt("dve", ld[:], ps[:], bc["decay_w0"][:], ALU.add, [psb, bwb], [B["ld"]])
                    act(ld[:], ld[:], AF.Sigmoid, [B["ld"]], [B["ld"]])
                    ts("dve", ld[:], ld[:], NEGE, None, ALU.mult, None, [B["ld"]], [B["ld"]])
                    yield
                    ps, psb, _ = prG.next()
                    mm(ps[:, :], lwT[0:64, 128:256], aup[:, :], True, True, [B["lwT"], bwb], [psb])
                    a = T["a"]
                    tt("dve", a[:], ps[:], bc["aaa_a0"][:], ALU.add, [psb, bwb], [B["a"]])
                    act(a[:], a[:], AF.Sigmoid, [B["a"]], [B["a"]])
                    yield
                    ps, psb, _ = prG.next()
                    mm(ps[:, :], lwT[:, 256:384], gup[:, :], True, True, [B["lwT"], bwb], [psb])
                    cp("act", T["gsb"][:], ps[:], [psb], [B["gsb"]])
                    kk, tmp, ss = T["kk"], T["tmp"], T["ss"]
                    tt("dve", kk[:], k, bc["k_k"][:], ALU.mult, [pb, bwb], [B["kk"]])
                    tt("dve", tmp[:], kk[:], kk[:], ALU.mult, [B["kk"]], [B["tmp"]])
                    S.op("dve", lambda e: e.tensor_reduce(ss[:, 0:8], tmp[:].rearrange("p (h d) -> p h d", d=64), AX.X, ALU.add),
                         reads=[B["tmp"]], writes=[B["ss"]])
                    ts("dve", ss[:, 0:8], ss[:, 0:8], 1e-24, None, ALU.max, None, [B["ss"]], [B["ss"]])
                    act(ss[:, 0:8], ss[:, 0:8], AF.Sqrt, [B["ss"]], [B["ss"]])
                    S.op("dve", lambda e: e.reciprocal(ss[:, 0:8], ss[:, 0:8]), reads=[B["ss"]], writes=[B["ss"]])
                    for h_ in range(8):
                        ts("dve", kk[:, h_ * 64:(h_ + 1) * 64], kk[:, h_ * 64:(h_ + 1) * 64], ss[:, h_:h_ + 1], None,
                           ALU.mult, None, [B["kk"], B["ss"]], [B["kk"]])
                    km, b_, bon = T["km"], T["b"], T["bon"]
                    stt(km[:], a[:], -1.0, bc["k_a"][:], ALU.add, ALU.mult, [B["a"], bwb], [B["km"]])
                    stt(km[:], km[:], 1.0, k, ALU.add, ALU.mult, [B["km"], pb], [B["km"]])
                    tt("dve", b_[:], kk[:], a[:], ALU.mult, [B["kk"], B["a"]], [B["b"]])
                    tt("dve", tmp[:], r, km[:], ALU.mult, [pb, B["km"], B["ss"]], [B["tmp"]])
                    tt("dve", tmp[:], tmp[:], bc["r_k"][:], ALU.mult, [B["tmp"], bwb], [B["tmp"]])
                    S.op("dve", lambda e: e.tensor_reduce(ss[:, 8:16], tmp[:].rearrange("p (h d) -> p h d", d=64), AX.X, ALU.add),
                         reads=[B["tmp"]], writes=[B["ss"]])
                    for h_ in range(8):
                        ts("dve", bon[:, h_ * 64:(h_ + 1) * 64], p[:, 1024 + h_ * 64:1024 + (h_ + 1) * 64], ss[:, 8 + h_:9 + h_],
                           None, ALU.mult, None, [pb, B["ss"]], [B["bon"]])
                    eL, eLm, enL, eD = T["eL"], T["eLm"], T["enL"], T["eD"]
                    yield
                    ps, psb, _ = prG.next()
                    mm(ps[:, :], CT["triinc"][:, :], ld[:, :], True, True, [B["ld"], cb], [psb])
                    act(eL[:], ps[:], AF.Exp, [psb], [B["eL"]])
                    act(enL[:], ps[:], AF.Exp, [psb], [B["enL"]], scale=-1.0)
                    tt("dve", eLm[:], ps[:], ld[:], ALU.subtract, [psb, B["ld"]], [B["eLm"]])
                    act(eLm[:], eLm[:], AF.Exp, [B["eLm"]], [B["eLm"]])
                    yield
                    ps, psb, _ = prG.next()
                    mm(ps[:, :], CT["trisu"][:, :], ld[:, :], True, True, [B["ld"], cb], [psb])
                    act(eD[:], ps[:], AF.Exp, [psb], [B["eD"]])
                    yield
                    ps, psb, _ = prG.next()
                    for h in range(8):
                        mm(ps[0:64, h:h + 1], ld[:, h * 64:(h + 1) * 64], onesf[:, 0:1], True, True, [B["ld"], cb], [psb])
                    act(T["PCs"][:, :], ps[0:64, 0:8], AF.Exp, [psb], [B["PCs"]])
                    yield
                    Q4, QT4 = T["Q4"], T["QT4"]
                    tt("dve", Q4[:, 0, :], kk[:], eLm[:], ALU.mult, [B["kk"], B["eLm"]], [B["Q4"]])
                    tt("dve", Q4[:, 1, :], r, eL[:], ALU.mult, [pb, B["eL"]], [B["Q4"]])
                    tt("dve", Q4[:, 2, :], b_[:], enL[:], ALU.mult, [B["b"], B["enL"]], [B["Q4"]])
                    tt("dve", Q4[:, 3, :], km[:], enL[:], ALU.mult, [B["km"], B["enL"]], [B["Q4"]])
                    tt("dve", T["Bp"][:], b_[:], eD[:], ALU.mult, [B["b"], B["eD"]], [B["Bp"]])
                    tt("dve", T["Kp"][:], km[:], eD[:], ALU.mult, [B["km"], B["eD"]], [B["Kp"]])
                    for h in range(8):
                        yield
                        p4, p4b, _ = prT.next()
                        for q in range(4):
                            tr(p4[0:64, q, :], Q4[:, q, h * 64:(h + 1) * 64], identf[:], [B["Q4"], cb], [p4b])
                        cp("act" if h % 2 == 0 else "dve", QT4[:, h, :, :], p4[:, :, :], [p4b], [B["QT4"]])
                    yield

                def tail(tI):
                    t0 = tI * 128
                    T = dict(T_)
                    T.update(sets[tI % 2][0])
                    B = dict(B_)
                    B.update(sets[tI % 2][1])
                    bon = T['bon']
                    ssg, tmpg = T['ssg'], T['tmpg']
                    ysb, yc = T["ysb"], T["yc"]
                    cp("act", ysb[:], yall[:], [byall], [B["ysb"]])
                    yield
                    ysb3 = ysb[:].rearrange("p (h d) -> p h d", d=64)
                    yc3 = yc[:].rearrange("p (h d) -> p h d", d=64)
                    S.op("dve", lambda e: e.tensor_reduce(ssg[:, 16:24], ysb3, AX.X, ALU.add), reads=[B["ysb"]], writes=[B["ssg"]])
                    ts("dve", ssg[:, 16:24], ssg[:, 16:24], 1.0 / 64.0, None, ALU.mult, None, [B["ssg"]], [B["ssg"]])
                    for h_ in range(8):
                        ts("dve", yc[:, h_ * 64:(h_ + 1) * 64], ysb[:, h_ * 64:(h_ + 1) * 64], ssg[:, 16 + h_:17 + h_], None,
                           ALU.subtract, None, [B["ysb"], B["ssg"]], [B["yc"]])
                    tt("dve", tmpg[:], yc[:], yc[:], ALU.mult, [B["yc"]], [B["tmpg"]])
                    yield
                    S.op("dve", lambda e: e.tensor_reduce(ssg[:, 16:24], tmpg[:].rearrange("p (h d) -> p h d", d=64), AX.X, ALU.add),
                         reads=[B["tmpg"], B["yc"]], writes=[B["ssg"]])
                    act(ssg[:, 16:24], ssg[:, 16:24], AF.Sqrt, [B["ssg"]], [B["ssg"]], scale=1.0 / 64.0, bias=GN_EPS)
                    S.op("dve", lambda e: e.reciprocal(ssg[:, 16:24], ssg[:, 16:24]), reads=[B["ssg"]], writes=[B["ssg"]])
                    yield
                    for h_ in range(8):
                        ts("dve", yc[:, h_ * 64:(h_ + 1) * 64], yc[:, h_ * 64:(h_ + 1) * 64], ssg[:, 16 + h_:17 + h_], None,
                           ALU.mult, None, [B["yc"], B["ssg"]], [B["yc"]])
                    tt("dve", yc[:], yc[:], bc["gn_w"][:], ALU.mult, [B["yc"], bwb], [B["yc"]])
                    tt("dve", yc[:], yc[:], bc["gn_b"][:], ALU.add, [B["yc"], bwb], [B["yc"]])
                    yield
                    tt("dve", yc[:], yc[:], bon[:], ALU.add, [B["yc"], B["bon"]], [B["yc"]])
                    tt("dve", yob[:], yc[:], T["gsb"][:], ALU.mult, [B["yc"], B["gsb"]], [byob])
                    yield
                    pb4, pb4b = v3(T4, 4, 128), bk[T4][1]
                    for j in range(4):
                        tr(pb4[:, j, :], yob[:, j * 128:(j + 1) * 128], identf[:], [byob, cb], [pb4b])
                    yat, yatb, _ = yatr.next()
                    cp("act", yat[:], pb4, [pb4b], [yatb])
                    yield
                    S.dma("sp", A["YAT"].rearrange("(c p) t -> p c t", p=128)[:, :, t0:t0 + 128], yat[:], reads=[yatb],
                          writes=[DT["YAT"]], key="st_yat")
                    yield

                lds[0] = m2_loads(0)
                if NT > 1:
                    lds[1] = m2_loads(1)
                for _ in prep(0):
                    pass
                tgen = iter(())
                for tI in range(NT):
                    t0 = tI * 128
                    T = dict(T_)
                    T.update(sets[tI % 2][0])
                    B = dict(B_)
                    B.update(sets[tI % 2][1])
                    p, pb, pv, pvb = lds[tI]
                    v = p[:, 1024:1536]
                    QT4 = T['QT4']
                    bon = T['bon']
                    gen = prep(tI + 1) if tI + 1 < NT else iter(())

                    def step():
                        next(gen, None)
                        next(tgen, None)
                    bq = B["QT4"]
                    A3, Ab = v3(A_, 4, 128), bk[A_][1]
                    B3, Bb = v3(B_k, 4, 128), bk[B_k][1]
                    C3, Cb = v3(C_, 4, 128), bk[C_][1]
                    SM3, SMb = v3(SM, 8, 64), bk[SM][1]
                    for hg in range(2):
                        hh = [4 * hg + j for j in range(4)]
                        AB4, AB4b, _ = AB4r.next()
                        AK4, AK4b, _ = AK4r.next()
                        for j, h in enumerate(hh):
                            kr2 = QT4[:, h, 0:2, :]
                            pak, pakb, _ = pakr.next()
                            mm(pak[:, 0:256], QT4[:, h, 2, :], kr2, True, True, [bq], [pakb])
                            mm(pak[:, 256:512], QT4[:, h, 3, :], kr2, True, True, [bq], [pakb])
                            tt("dve", AB4[:, j, :], pak[:, 0:256], CT["mAB"][:], ALU.mult, [pakb, cb], [AB4b])
                            tt("dve", AK4[:, j, :], pak[:, 256:512], CT["mAK"][:], ALU.mult, [pakb, cb], [AK4b])
                        for j, h in enumerate(hh):
                            mm(C3[:, j, :], QT4[:, h, 0, :], QT4[:, h, 2, :], True, True, [bq], [Cb])
                        step()
                        Y4, Y4b, _ = Y4r.next()
                        tt("dve", Y4[:], C3, mN4, ALU.mult, [Cb, cb], [Y4b])
                        TT4, TT4b, _ = TT4r.next()
                        tt("dve", TT4[:], AB4[:, :, 0:128], id4, ALU.add, [AB4b, cb], [TT4b])
                        Zt, Zb, zab = AB4, AB4b, True
                        for lv in range(6):
                            step()
                            def zv(j):
                                return Zt[:, j, 0:128] if zab else Zt[:, j, :]
                            if lv < 5:
                                for j in range(4):
                                    mm(A3[:, j, :], Y4[:, j, :], zv(j), True, True, [Y4b, Zb], [Ab])
                            for j in range(4):
                                mm(B3[:, j, :], zv(j), Y4[:, j, :], True, True, [Y4b, Zb], [Bb])
                            Yn4, Yn4b, _ = Y4r.next()
                            cp("act", Yn4[:], B3, [Bb], [Yn4b])
                            if lv < 5:
                                Zn4, Zn4b, _ = Z4r.next()
                                cp("dve", Zn4[:], A3, [Ab], [Zn4b])
                            for j in range(4):
                                mm(C3[:, j, :], Yn4[:, j, :], TT4[:, j, :], True, True, [Yn4b, TT4b], [Cb])
                            TTn4, TTn4b, _ = TT4r.next()
                            tt("dve", TTn4[:], C3, TT4[:], ALU.add, [Cb, TT4b], [TTn4b])
                            TT4, TT4b = TTn4, TTn4b
                            Y4, Y4b = Yn4, Yn4b
                            if lv < 5:
                                Zt, Zb, zab = Zn4, Zn4b, False
                        step()
                        for j, h in enumerate(hh):
                            hs = slice(h * 64, (h + 1) * 64)
                            mm(SM3[:, j, :], QT4[:, h, 0, :], ST[:, h, :], True, False, [bq, stb[h]], [SMb])
                            mm(SM3[:, j, :], AK4[:, j, 0:128], v[:, hs], False, True, [AK4b, pb], [SMb])
                        W4, W4b, _ = W4r.next()
                        cp("act", W4[:], SM3[:, 0:4, :], [SMb], [W4b])
                        for j, h in enumerate(hh):
                            mm(SM3[:, 4 + j, :], TT4[:, j, :], W4[:, j, :], True, True, [TT4b, W4b], [SMb])
                        U4, U4b, _ = U4r.next()
                        act(U4[:], SM3[:, 4:8, :], AF.Copy, [SMb], [U4b], scale=-1.0)
                        step()
                        for j, h in enumerate(hh):
                            hs = slice(h * 64, (h + 1) * 64)
                            mm(yall[:, hs], QT4[:, h, 1, :], ST[:, h, :], True, False, [bq, stb[h]], [byall])
                            mm(yall[:, hs], AB4[:, j, 128:256], U4[:, j, :], False, False, [AB4b, U4b], [byall])
                            mm(yall[:, hs], AK4[:, j, 128:256], v[:, hs], False, True, [AK4b, pb], [byall])
                        step()
                        for j, h in enumerate(hh):
                            hs = slice(h * 64, (h + 1) * 64)
                            mm(SM3[0:64, j, :], T["Bp"][:, hs], U4[:, j, :], True, False, [B["Bp"], U4b], [SMb])
                            mm(SM3[0:64, j, :], T["Kp"][:, hs], v[:, hs], False, True, [B["Kp"], pb], [SMb])
                        for j, h in enumerate(hh):
                            stt(ST[:, h, :], ST[:, h, :], T["PCs"][:, h:h + 1], SM3[0:64, j, :], ALU.mult, ALU.add,
                                [stb[h], B["PCs"], SMb], [stb[h]])
                    for _ in gen:
                        pass
                    for _ in tgen:
                        pass
                    if tI + 2 < NT:
                        lds[tI + 2] = m2_loads(tI + 2)
                    tgen = tail(tI)
                    next(tgen)
                for _ in tgen:
                    pass
            except _StopM2:
                pass
            S.barrier()

    def stage_m3(l):
        with ExitStack() as es:
            qr = Ring(es, nc, "m3q", (96, S_), BF16, 2)
            kr_ = Ring(es, nc, "m3k", (96, S_), BF16, 2)
            vr = Ring(es, nc, "m3v", (128, NT, 128), BF16, 2)
            psS = Ring(es, nc, "m3ps", (128, 512), F32, 4, psum=True)
            psO = Ring(es, nc, "m3po", (128, 512), F32, 2, psum=True)
            ptr_ = Ring(es, nc, "m3pt", (128, 512), BF16, 3)
            rcr = Ring(es, nc, "m3rc", (64, 512), F32, 2)
            ybr = Ring(es, nc, "m3yb", (64, 512), BF16, 2)
            def m3_loads(hh_):
                qT_, qb_, qk = qr.next()
                kT_, kb_, kk_ = kr_.next()
                va_, vb_, vk = vr.next()
                S.dma("sp", qT_[:], A["QT"][hh_], reads=[DT["QT"]], writes=[qb_], key=qk)
                S.dma("sp", kT_[:], A["KT"][hh_], reads=[DT["KT"]], writes=[kb_], key=kk_)
                S.dma("sp", va_[:], A["VA"][hh_].rearrange("(n p) d -> p n d", p=128), reads=[DT["VA"]], writes=[vb_], key=vk)
                return qT_, qb_, kT_, kb_, va_, vb_

            nxt3 = m3_loads(0)
            for h in range(8):
                qT, qb, kT, kb, va, vb = nxt3
                if h + 1 < 8:
                    nxt3 = m3_loads(h + 1)
                for g in range(NG):
                    gs = slice(g * 512, (g + 1) * 512)
                    po, pob, _ = psO.next()
                    nj = 4 * g + 4

                    def m3_scores(j):
                        ps_, psb_, _ = psS.next()
                        diag = j >= 4 * g
                        mm(ps_[:, :], kT[0:96, j * 128:(j + 1) * 128], qT[0:96, gs], True, not diag, [kb, qb], [psb_])
                        if diag:
                            mm(ps_[:, :], identb[:, :], CT["cmask"][:, j - 4 * g, :], False, True, [cb], [psb_])
                        return ps_, psb_

                    sq_ = [m3_scores(0)]
                    if nj > 1:
                        sq_.append(m3_scores(1))
                    for j in range(nj):
                        ps, psb = sq_.pop(0)
                        if j + 2 < nj:
                            sq_.append(m3_scores(j + 2))
                        pt, ptb, _ = ptr_.next()
                        act(pt[:], ps[:], AF.Exp, [psb], [ptb])
                        mm(po[:, :], va[:, j, :], pt[:], j == 0, j == nj - 1, [vb, ptb], [pob])
                    rc, rcb, _ = rcr.next()
                    yb, ybb, _ = ybr.next()
                    S.op("dve", lambda e: e.reciprocal(rc[0:64, :], po[64:128, :]), reads=[pob], writes=[rcb])
                    tt("dve", yb[0:64, :], po[0:64, :], rc[0:64, :], ALU.mult, [pob, rcb], [ybb])
                    S.dma("sp", A["YBT"][h * 64:(h + 1) * 64, gs], yb[:], reads=[ybb], writes=[DT["YBT"]], key="st_ybt")
            S.barrier()

    def stage_m4(l, Xsrc, xsb, Xdst, xdb):
        with ExitStack() as es:
            pa = sb(es, "pa", (128, 4, 1024), BF16)
            pb_ = sb(es, "pb", (128, 4, 1024), BF16)
            wo = sb(es, "wo", (128, 8, 1024), BF16)
            g1 = sb(es, "g1bc", (128, 1024))
            lnw = sb(es, "lnw", (128, 1024))
            lnb = sb(es, "lnb", (128, 1024))
            bw, bcb = TB(), TB()
            S.dma("pool", pa[:], A["p_a"][l].rearrange("(c p) n -> p c n", p=128), writes=[bw], key="w_m4")
            S.dma("pool", pb_[:], A["p_b"][l].rearrange("(c p) n -> p c n", p=128), writes=[bw], key="w_m4")
            S.dma("pool", wo[:], A["w_o"][l].rearrange("(c p) n -> p c n", p=128), writes=[bw], key="w_m4")
            S.dma("sp", g1[:], A["MOD"][l, 2048:3072].partition_broadcast(128), reads=[DT["MOD"]], writes=[bcb], key="w_m4b")
            S.dma("sp", lnw[:], A["ln1_w"][l].partition_broadcast(128), writes=[bcb], key="w_m4b")
            S.dma("sp", lnb[:], A["ln1_b"][l].partition_broadcast(128), writes=[bcb], key="w_m4b")
            R = {"st": Ring(es, nc, "m4st", (128, 16), F32, 4)}
            yar = Ring(es, nc, "m4ya", (128, 4, 512), BF16, 2)
            ybr = Ring(es, nc, "m4yb", (128, 4, 512), BF16, 2)
            gar = Ring(es, nc, "m4ga", (128, 512), F32, 3)
            gbr = Ring(es, nc, "m4gb", (128, 512), F32, 3)
            t1r = Ring(es, nc, "m4t1", (128, 512), F32, 2)
            t2r = Ring(es, nc, "m4t2", (128, 512), F32, 2)
            mTr = Ring(es, nc, "m4mT", (128, 8, 512), BF16, 2)
            xr = Ring(es, nc, "m4x", (128, 1024), F32, 3)
            fr = Ring(es, nc, "m4f", (128, 1024), F32, 3)
            psA = Ring(es, nc, "m4ps", (128, 512), F32, 6, psum=True)
            yav = A["YAT"].rearrange("(c p) t -> p c t", p=128)
            ybv = A["YBT"].rearrange("(c p) t -> p c t", p=128)
            for g in range(NG):
                gs = slice(g * 512, (g + 1) * 512)
                ya, yab, yak = yar.next()
                yb, ybb, ybk = ybr.next()
                S.dma("sp", ya[:], yav[:, :, gs], reads=[DT["YAT"]], writes=[yab], key=yak)
                S.dma("sp", yb[:], ybv[:, :, gs], reads=[DT["YBT"]], writes=[ybb], key=ybk)
                mT, mTb, _ = mTr.next()
                for fc in range(8):
                    fs = slice(fc * 128, (fc + 1) * 128)
                    ga, gab, gak = gar.next()
                    gb, gbb, gbk = gbr.next()
                    S.dma("sp", ga[:], A["GT"][fc * 128:(fc + 1) * 128, gs], reads=[DT["GT"]], writes=[gab], key=gak)
                    S.dma("sp", gb[:], A["GT"][1024 + fc * 128:1024 + (fc + 1) * 128, gs], reads=[DT["GT"]], writes=[gbb], key=gbk)
                    p1, p1b, _ = psA.next()
                    for c in range(4):
                        mm(p1[:, :], pa[:, c, fs], ya[:, c, :], c == 0, c == 3, [bw, yab], [p1b])
                    p2, p2b, _ = psA.next()
                    for c in range(4):
                        mm(p2[:, :], pb_[:, c, fs], yb[:, c, :], c == 0, c == 3, [bw, ybb], [p2b])
                    a1, a1b, _ = t1r.next()
                    a2, a2b, _ = t2r.next()
                    tt("dve", a1[:], p1[:], ga[:], ALU.mult, [p1b, gab], [a1b])
                    tt("dve", a2[:], p2[:], gb[:], ALU.mult, [p2b, gbb], [a2b])
                    tt("pool", mT[:, fc, :], a1[:], a2[:], ALU.add, [a1b, a2b], [mTb])
                for t4 in range(4):
                    t0 = g * 512 + t4 * 128
                    xt, xb, xk = xr.next()
                    S.dma("sp", xt[:], Xsrc[t0:t0 + 128, :], reads=[xsb], writes=[xb], key=xk)
                    f, fb, _ = fr.next()
                    for cg in range(2):
                        pm, pmb, _ = psA.next()
                        for c in range(8):
                            mm(pm[:, :], mT[:, c, t4 * 128:(t4 + 1) * 128], wo[:, c, cg * 512:(cg + 1) * 512], c == 0, c == 7,
                               [mTb, bw], [pmb])
                        tt("dve", f[:, cg * 512:(cg + 1) * 512], pm[:], g1[:, cg * 512:(cg + 1) * 512], ALU.mult,
                           [pmb, bcb], [fb])
                    resid_ln(R, f, fb, xt, xb, lnw, lnb, bcb, Xdst, xdb, t0, "st_x")
            S.barrier()

    def stage_ffn(l, Xsrc, xsb, Xdst, xdb, moe):
        F_ = DFFE if moe else DFF
        FB = 512 if moe else 256
        nch = FB // 128
        nblk = F_ // FB
        nexp = NE if moe else 1
        li = l // 2
        with ExitStack() as es:
            g2 = sb(es, "g2bc", (128, 1024))
            lnw = sb(es, "lnw2", (128, 1024))
            lnb = sb(es, "lnb2", (128, 1024))
            bcb = TB()
            S.dma("sp", g2[:], A["MOD"][l, 5120:6144].partition_broadcast(128), reads=[DT["MOD"]], writes=[bcb], key="w_f")
            S.dma("sp", lnw[:], A["ln2_w"][l].partition_broadcast(128), writes=[bcb], key="w_f")
            S.dma("sp", lnb[:], A["ln2_b"][l].partition_broadcast(128), writes=[bcb], key="w_f")
            R = {"x": Ring(es, nc, "fx", (128, 1024), F32, 6), "st": Ring(es, nc, "fst", (128, 16), F32, 4),
                 "xn": Ring(es, nc, "fxn", (128, 1024), BF16, 2),
                 "ptr": Ring(es, nc, "fptr", (128, 1024), BF16, 1, psum=True)}
            hTr = Ring(es, nc, "hT", (128, 8, 512), BF16, 2)
            wgr = Ring(es, nc, "wg" if moe else "wgd", (128, 8, FB), BF16, 2)
            wur = Ring(es, nc, "wu" if moe else "wud", (128, 8, FB), BF16, 2)
            wdr = Ring(es, nc, "wd" if moe else "wdd", (128, nch, 1024), BF16, 2)
            if not moe:
                for nm, src in (("WG", "ffn_w_gate"), ("WU", "ffn_w_up"), ("WD", "ffn_w_down")):
                    S.dma("pool", A[nm].rearrange("(p r) n -> p (r n)", p=128),
                          A[src][li].rearrange("(p r) n -> p (r n)", p=128), writes=[DT[nm]], key="pre")
            psG = Ring(es, nc, "fpsG", (128, 512), F32, 2, psum=True)
            psU = Ring(es, nc, "fpsU", (128, 512), F32, 2, psum=True)
            psD = Ring(es, nc, "fpsD", (128, 512), F32, 2 if moe else 3, psum=True)
            sgr = Ring(es, nc, "fsg", (128, 512), F32, 2)
            aTr = Ring(es, nc, "faT", (128, nch, 512), BF16, 2)
            accr = Ring(es, nc, "facc", (128, 1024), F32, 8)
            if moe:
                rw = sb(es, "rw", (128, 8, 8))
                rbb = sb(es, "rbb", (128, 8))
                S.dma("sp", rw[:], A["router_w"][li].rearrange("(c p) e -> p c e", p=128), writes=[bcb], key="w_f")
                S.dma("sp", rbb[:], A["router_b"][li].partition_broadcast(128), writes=[bcb], key="w_f")
                x32r = Ring(es, nc, "fx32", (128, 1024), F32, 2)
                h32r = Ring(es, nc, "fh32", (128, 8, 128), F32, 2)
                p32r = Ring(es, nc, "fp32", (128, 512), F32, 1, psum=True)
                lgr = Ring(es, nc, "flg", (128, 48), F32, 8)
            for g in range(NG):
                hT, hb, _ = hTr.next()
                gws = []

                def router(t4, xt, xb, st, stb):
                    x32, x32b, _ = x32r.next()
                    ts("dve", x32[:], xt[:], st[:, 12:13], st[:, 14:15], ALU.subtract, ALU.mult, [xb, stb], [x32b])
                    h32, h32b, _ = h32r.next()
                    for half in range(2):
                        pp, ppb, _ = p32r.next()
                        for c4 in range(4):
                            c = half * 4 + c4
                            tr(pp[:, c4 * 128:(c4 + 1) * 128], x32[:, c * 128:(c + 1) * 128], identf[:], [x32b, cb], [ppb])
                        for c4 in range(4):
                            c = half * 4 + c4
                            act(h32[:, c, :], pp[:, c4 * 128:(c4 + 1) * 128], AF.Identity, [ppb, bmodc], [h32b],
                                scale=modc[:, 32 + c:33 + c], bias=modc[:, 24 + c:25 + c])
                    pp, ppb, _ = p32r.next()
                    for c in range(8):
                        mm(pp[:, 0:8], h32[:, c, :], rw[:, c, :], c == 0, c == 7, [h32b, bcb], [ppb])
                    lg, lgb, _ = lgr.next()
                    L_ = lg[:, 0:8]
                    tt("dve", L_, pp[:, 0:8], rbb[:], ALU.add, [ppb, bcb], [lgb])
                    S.op("dve", lambda e: e.tensor_reduce(lg[:, 40:41], L_, AX.X, ALU.max), reads=[lgb], writes=[lgb])
                    ts("dve", lg[:, 8:16], L_, lg[:, 40:41], None, ALU.is_equal, None, [lgb], [lgb])
                    stt(lg[:, 16:24], lg[:, 8:16], -1e30, L_, ALU.mult, ALU.add, [lgb], [lgb])
                    S.op("dve", lambda e: e.tensor_reduce(lg[:, 41:42], lg[:, 16:24], AX.X, ALU.max), reads=[lgb], writes=[lgb])
                    ts("dve", lg[:, 24:32], lg[:, 16:24], lg[:, 41:42], None, ALU.is_equal, None, [lgb], [lgb])
                    tt("dve", lg[:, 42:43], lg[:, 40:41], lg[:, 41:42], ALU.subtract, [lgb], [lgb])
                    act(lg[:, 43:44], lg[:, 42:43], AF.Sigmoid, [lgb], [lgb])
                    act(lg[:, 44:45], lg[:, 42:43], AF.Sigmoid, [lgb], [lgb], scale=-1.0)
                    ts("dve", lg[:, 32:40], lg[:, 8:16], lg[:, 43:44], None, ALU.mult, None, [lgb], [lgb])
                    stt(lg[:, 32:40], lg[:, 24:32], lg[:, 44:45], lg[:, 32:40], ALU.mult, ALU.add, [lgb], [lgb])
                    gws.append((lg, lgb))

                xs = lnt(R, Xsrc, xsb, g, 32, 24, hT, hb, want32=router if moe else None)
                accs = [accr.next() for _ in range(4)]
                first = True
                for e_ in range(nexp):
                    if moe:
                        wgv = A["moe_w_gate"][li, e_].rearrange("(c p) n -> p c n", p=128)
                        wuv = A["moe_w_up"][li, e_].rearrange("(c p) n -> p c n", p=128)
                        wdv = A["moe_w_down"][li, e_].rearrange("(c p) n -> p c n", p=128)
                    else:
                        wgv = A["WG"].rearrange("(c p) n -> p c n", p=128)
                        wuv = A["WU"].rearrange("(c p) n -> p c n", p=128)
                        wdv = A["WD"].rearrange("(c p) n -> p c n", p=128)
                    for bi in range(nblk):
                        wg, wgb, wgk = wgr.next()
                        wu, wub, wuk = wur.next()
                        wd, wdb, wdk = wdr.next()
                        wq_ = "pool" if moe else "sp"
                        wrd = [] if moe else [DT["WG"], DT["WU"], DT["WD"]]
                        S.dma(wq_, wg[:], wgv[:, :, bi * FB:(bi + 1) * FB], reads=wrd[0:1], writes=[wgb], key=wgk)
                        S.dma(wq_, wu[:], wuv[:, :, bi * FB:(bi + 1) * FB], reads=wrd[1:2], writes=[wub], key=wuk)
                        S.dma(wq_, wd[:], wdv[:, bi * nch:(bi + 1) * nch, :], reads=wrd[2:3], writes=[wdb], key=wdk)
                        aT, aTb, _ = aTr.next()
                        for fc in range(nch):
                            pg, pgb, _ = psG.next()
                            for c in range(8):
                                mm(pg[:, :], wg[:, c, fc * 128:(fc + 1) * 128], hT[:, c, :], c == 0, c == 7, [wgb, hb], [pgb])
                            pu, pub, _ = psU.next()
                            for c in range(8):
                                mm(pu[:, :], wu[:, c, fc * 128:(fc + 1) * 128], hT[:, c, :], c == 0, c == 7, [wub, hb], [pub])
                            sg, sgb, _ = sgr.next()
                            act(sg[:], pg[:], AF.Silu, [pgb], [sgb])
                            tt("dve", aT[:, fc, :], sg[:], pu[:], ALU.mult, [sgb, pub], [aTb])
                        for t4 in range(4):
                            acc, accb, _ = accs[t4]
                            for cg in range(2):
                                pd, pdb, _ = psD.next()
                                for fc in range(nch):
                                    mm(pd[:, :], aT[:, fc, t4 * 128:(t4 + 1) * 128], wd[:, fc, cg * 512:(cg + 1) * 512],
                                       fc == 0, fc == nch - 1, [aTb, wdb], [pdb])
                                cs_ = slice(cg * 512, (cg + 1) * 512)
                                if moe:
                                    lg, lgb = gws[t4]
                                    if first:
                                        ts("dve", acc[:, cs_], pd[:], lg[:, 32 + e_:33 + e_], None, ALU.mult, None,
                                           [pdb, lgb], [accb])
                                    else:
                                        stt(acc[:, cs_], pd[:], lg[:, 32 + e_:33 + e_], acc[:, cs_], ALU.mult, ALU.add,
                                            [pdb, lgb, accb], [accb])
                                else:
                                    if first:
                                        cp("act", acc[:, cs_], pd[:], [pdb], [accb])
                                    else:
                                        tt("dve", acc[:, cs_], pd[:], acc[:, cs_], ALU.add, [pdb, accb], [accb])
                        first = False
                for t4 in range(4):
                    t0 = g * 512 + t4 * 128
                    acc, accb, _ = accs[t4]
                    xt, xb = xs[t4]
                    tt("pool", acc[:], acc[:], g2[:], ALU.mult, [accb, bcb], [accb])
                    resid_ln(R, acc, accb, xt, xb, lnw, lnb, bcb, Xdst, xdb, t0, "st_x")
            S.barrier()

    import os
    STOP = os.environ.get("MK_STOP", "")
    cur, curb = A["x"], DT["x"]
    for l in range(NL):
        if STOP == "ada":
            break
        layer_setup(l)
        if STOP == "setup":
            break
        stage_m1(l, cur, curb)
        if STOP == "m1":
            break
        stage_m2(l)
        if STOP == "m2":
            break
        stage_m3(l)
        if STOP == "m3":
            break
        stage_m4(l, cur, curb, A["XB"], DT["XB"])
        if STOP == "m4":
            break
        last = (l == NL - 1)
        dst, dstb = (Y, DT["y"]) if last else (A["XA"], DT["XA"])
        stage_ffn(l, A["XB"], DT["XB"], dst, dstb, moe=(l % 2 == 1))
        cur, curb = A["XA"], DT["XA"]
    S.barrier()
    G.close()
    nsem = len(S.sems)
    S.close()
    return nc, nsem, S.nins


_CACHE = {}


def run(inputs, S_, NL, ncores, dbg=()):
    key = (S_, NL, tuple(dbg))
    if key not in _CACHE:
        _CACHE[key] = build(S_, NL, dbg)
    nc, nsem, nins = _CACHE[key]
    consts = host_consts()
    shared = {}
    for n, s in WSHAPES:
        a = np.ascontiguousarray(inputs[n], dtype=np.float32)
        shared[n] = a.reshape(s)
    shared.update(consts)
    in_maps = []
    for b in range(ncores):
        m = dict(shared)
        m["x"] = np.ascontiguousarray(inputs["x"][b], dtype=np.float32)
        m["c"] = np.ascontiguousarray(inputs["c"][b], dtype=np.float32).reshape(8, 128)
        m["pos"] = np.ascontiguousarray(inputs["positions"][b], dtype=np.int32)
        in_maps.append(m)
    res = run_bass_kernel_spmd(nc, in_maps, core_ids=list(range(ncores)))
    return res


def kernel(**inputs):
    inputs = {k: np.asarray(v) for k, v in inputs.items()}
    B, S_, _ = inputs["x"].shape
    res = run(inputs, S_, 4, B)
    return np.stack([res.results[b]["y"] for b in range(B)], axis=0).astype(np.float32)
```
